# Optimizing a Trainium2 kernel written in Bass

```python
import math
import jax, jax.numpy as jnp
from jax import lax
import numpy as np

D_MODEL = 2048
BATCH = 16
SEQ = 2048
DEPTH = 1

N_DIFF_HEADS = 8
DIFF_HEAD_DIM = 128
DIFF_QK_HALF = DIFF_HEAD_DIM // 2
N_DSA_HEADS = 8
DSA_HEAD_DIM = 128
N_IDX_HEADS = 16
IDX_HEAD_DIM = 64
DSA_TOPK_MAX = 256
PEER_HEADS = 8
PEER_NKEYS = 128
PEER_N_EXPERTS = PEER_NKEYS * PEER_NKEYS
PEER_QDIM = 256
PEER_HALF = PEER_QDIM // 2
PEER_TOPK = 16
Q_BLOCK = 128
TOKEN_CHUNK = 128
LN_EPS = 1e-5
RMS_EPS = 1e-5
ALPHA = (2 * DEPTH) ** 0.25
BETA = (8 * DEPTH) ** -0.25

DIFF_W = N_DIFF_HEADS * DIFF_HEAD_DIM
DSA_QW = N_DSA_HEADS * DSA_HEAD_DIM
IDX_QW = N_IDX_HEADS * IDX_HEAD_DIM
MIX_W = DIFF_W + DSA_QW
IN_SPLITS = (DIFF_W, DIFF_W, DIFF_W, DSA_QW, DSA_HEAD_DIM, DSA_HEAD_DIM, IDX_QW, IDX_HEAD_DIM, N_IDX_HEADS)
IN_IS_VALUE = (False, False, True, False, False, True, False, False, False)
IN_COLS = sum(IN_SPLITS)

kernel_name = "hymba_diffattn_dsa_peer_deepnorm"


def _alibi_slopes():
    n = N_DIFF_HEADS + N_DSA_HEADS
    s = 2.0 ** (-8.0 * np.arange(1, n + 1) / n)
    return (jnp.asarray(s[0::2], dtype=jnp.float32), jnp.asarray(s[1::2], dtype=jnp.float32))


def _layer_norm(x, g, b):
    xf = x.astype(jnp.float32)
    mu = jnp.mean(xf, axis=-1, keepdims=True)
    var = jnp.mean(jnp.square(xf - mu), axis=-1, keepdims=True)
    return ((xf - mu) * lax.rsqrt(var + LN_EPS) * g.astype(jnp.float32) + b.astype(jnp.float32)).astype(x.dtype)


def _split_columns(proj):
    offsets = [int(o) for o in np.cumsum(IN_SPLITS)[:-1]]
    return jnp.split(proj, offsets, axis=-1)


def _diff_attention(q, k, v, lam, lam_init, subln_g, slopes):
    B, S = q.shape[0], q.shape[1]
    scale = DIFF_QK_HALF ** -0.5
    kpos = jnp.arange(S)

    def block(i):
        qs = lax.dynamic_slice_in_dim(q, i * Q_BLOCK, Q_BLOCK, axis=1)
        qpos = i * Q_BLOCK + jnp.arange(Q_BLOCK)
        logits = jnp.einsum('bqhcd,bshcd->bchqs', qs, k).astype(jnp.float32) * scale
        dist = (qpos[:, None] - kpos[None, :]).astype(jnp.float32)
        logits = jnp.where(dist >= 0, logits - slopes[:, None, None] * dist, -jnp.inf)
        p = jax.nn.softmax(logits, axis=-1)
        w = p[:, 0] - lam * p[:, 1]
        return jnp.einsum('bhqs,bshd->bqhd', w.astype(v.dtype), v)

    out = lax.map(block, jnp.arange(S // Q_BLOCK))
    out = jnp.moveaxis(out, 0, 1).reshape(B, S, N_DIFF_HEADS, DIFF_HEAD_DIM)
    of = out.astype(jnp.float32)
    of = of * lax.rsqrt(jnp.mean(jnp.square(of), axis=-1, keepdims=True) + RMS_EPS)
    of = of * subln_g.astype(jnp.float32) * (1.0 - lam_init)
    return of.astype(v.dtype)


def _dsa_attention(q, k, v, iq, ik, iw, slopes):
    B, S = q.shape[0], q.shape[1]
    topk = min(DSA_TOPK_MAX, S // 4)
    scale = DSA_HEAD_DIM ** -0.5
    idx_scale = (IDX_HEAD_DIM ** -0.5) * (N_IDX_HEADS ** -0.5)
    kpos = jnp.arange(S)
    gather = jax.vmap(lambda t, idx: t[idx])

    def block(i):
        qs = lax.dynamic_slice_in_dim(q, i * Q_BLOCK, Q_BLOCK, axis=1)
        iqs = lax.dynamic_slice_in_dim(iq, i * Q_BLOCK, Q_BLOCK, axis=1)
        iws = lax.dynamic_slice_in_dim(iw, i * Q_BLOCK, Q_BLOCK, axis=1)
        qpos = i * Q_BLOCK + jnp.arange(Q_BLOCK)
        rel = jax.nn.relu(jnp.einsum('bqhd,bsd->bqhs', iqs, ik).astype(jnp.float32))
        iscore = jnp.einsum('bqh,bqhs->bqs', iws.astype(jnp.float32), rel) * idx_scale
        iscore = jnp.where(kpos[None, :] <= qpos[:, None], iscore, -jnp.inf)
        _, sel = lax.top_k(iscore, topk)
        ks = gather(k, sel)
        vs = gather(v, sel)
        logits = jnp.einsum('bqhd,bqkd->bhqk', qs, ks).astype(jnp.float32) * scale
        dist = (qpos[None, :, None] - sel).astype(jnp.float32)
        logits = jnp.where((dist >= 0)[:, None], logits - slopes[None, :, None, None] * dist[:, None], -jnp.inf)
        p = jax.nn.softmax(logits, axis=-1)
        return jnp.einsum('bhqk,bqkd->bqhd', p.astype(vs.dtype), vs)

    out = lax.map(block, jnp.arange(S // Q_BLOCK))
    return jnp.moveaxis(out, 0, 1).reshape(B, S, N_DSA_HEADS, DSA_HEAD_DIM)


def _peer(x, wq, sk1, sk2, u, v):
    B, S, D = x.shape
    xt = x.reshape((B * S) // TOKEN_CHUNK, TOKEN_CHUNK, D)

    def chunk(xc):
        qh = (xc @ wq).reshape(TOKEN_CHUNK, PEER_HEADS, 2, PEER_HALF)
        s1 = jnp.einsum('chd,nd->chn', qh[:, :, 0], sk1).astype(jnp.float32)
        s2 = jnp.einsum('chd,nd->chn', qh[:, :, 1], sk2).astype(jnp.float32)
        v1, i1 = lax.top_k(s1, PEER_TOPK)
        v2, i2 = lax.top_k(s2, PEER_TOPK)
        cand = (v1[..., :, None] + v2[..., None, :]).reshape(TOKEN_CHUNK, PEER_HEADS, PEER_TOPK * PEER_TOPK)
        cidx = (i1[..., :, None] * PEER_NKEYS + i2[..., None, :]).reshape(TOKEN_CHUNK, PEER_HEADS, PEER_TOPK * PEER_TOPK)
        top_s, pos = lax.top_k(cand, PEER_TOPK)
        eidx = jnp.take_along_axis(cidx, pos, axis=-1)
        g = jax.nn.softmax(top_s, axis=-1)
        ue = u[eidx]
        h = jnp.einsum('cd,chkd->chk', xc, ue).astype(jnp.float32)
        a = jax.nn.gelu(h, approximate=False) * g
        ve = v[eidx]
        return jnp.einsum('chk,chkd->cd', a.astype(ve.dtype), ve)

    return lax.map(chunk, xt).reshape(B, S, D)


def setup_inputs(seed: int = 0) -> dict:
    key = jax.random.key(seed)
    ks = jax.random.split(key, 20)
    f32 = jnp.float32
    col_scale = jnp.asarray(np.concatenate([np.full(n, BETA if isv else 1.0) for n, isv in zip(IN_SPLITS, IN_IS_VALUE)]).astype(np.float32))
    x = jax.random.normal(ks[0], (BATCH, SEQ, D_MODEL), f32)
    w_in = jax.random.normal(ks[1], (DEPTH, D_MODEL, IN_COLS), f32) * (D_MODEL ** -0.5) * col_scale
    w_o = jax.random.normal(ks[2], (DEPTH, MIX_W, D_MODEL), f32) * (MIX_W ** -0.5) * BETA
    lambda_q1 = jax.random.normal(ks[3], (DEPTH, DIFF_QK_HALF), f32) * 0.1
    lambda_k1 = jax.random.normal(ks[4], (DEPTH, DIFF_QK_HALF), f32) * 0.1
    lambda_q2 = jax.random.normal(ks[5], (DEPTH, DIFF_QK_HALF), f32) * 0.1
    lambda_k2 = jax.random.normal(ks[6], (DEPTH, DIFF_QK_HALF), f32) * 0.1
    subln_g = 1.0 + 0.02 * jax.random.normal(ks[7], (DEPTH, DIFF_HEAD_DIM), f32)
    ln1_g = 1.0 + 0.02 * jax.random.normal(ks[8], (DEPTH, D_MODEL), f32)
    ln1_b = 0.02 * jax.random.normal(ks[9], (DEPTH, D_MODEL), f32)
    peer_wq = jax.random.normal(ks[10], (DEPTH, D_MODEL, PEER_HEADS * PEER_QDIM), f32) * (D_MODEL ** -0.5)
    peer_k1 = jax.random.normal(ks[11], (DEPTH, PEER_NKEYS, PEER_HALF), f32) * (PEER_HALF ** -0.5)
    peer_k2 = jax.random.normal(ks[12], (DEPTH, PEER_NKEYS, PEER_HALF), f32) * (PEER_HALF ** -0.5)
    peer_u = jax.random.normal(ks[13], (DEPTH, PEER_N_EXPERTS, D_MODEL), f32) * (D_MODEL ** -0.5) * BETA
    peer_v = jax.random.normal(ks[14], (DEPTH, PEER_N_EXPERTS, D_MODEL), f32) * BETA
    ln2_g = 1.0 + 0.02 * jax.random.normal(ks[15], (DEPTH, D_MODEL), f32)
    ln2_b = 0.02 * jax.random.normal(ks[16], (DEPTH, D_MODEL), f32)
    return {"x": x, "w_in": w_in, "w_o": w_o, "lambda_q1": lambda_q1, "lambda_k1": lambda_k1,
            "lambda_q2": lambda_q2, "lambda_k2": lambda_k2, "subln_g": subln_g, "ln1_g": ln1_g,
            "ln1_b": ln1_b, "peer_wq": peer_wq, "peer_k1": peer_k1, "peer_k2": peer_k2,
            "peer_u": peer_u, "peer_v": peer_v, "ln2_g": ln2_g, "ln2_b": ln2_b}


def reference(x, w_in, w_o, lambda_q1, lambda_k1, lambda_q2, lambda_k2, subln_g, ln1_g, ln1_b,
              peer_wq, peer_k1, peer_k2, peer_u, peer_v, ln2_g, ln2_b):
    B, S, _ = x.shape
    slopes_diff, slopes_dsa = _alibi_slopes()
    for l in range(DEPTH):
        proj = x @ w_in[l]
        dq, dk, dv, sq, sk, sv, iq, ik, iw = _split_columns(proj)
        lam_init = 0.8 - 0.6 * math.exp(-0.3 * l)
        lam = (jnp.exp(jnp.sum(lambda_q1[l].astype(jnp.float32) * lambda_k1[l].astype(jnp.float32)))
               - jnp.exp(jnp.sum(lambda_q2[l].astype(jnp.float32) * lambda_k2[l].astype(jnp.float32)))
               + lam_init)
        diff_out = _diff_attention(
            dq.reshape(B, S, N_DIFF_HEADS, 2, DIFF_QK_HALF),
            dk.reshape(B, S, N_DIFF_HEADS, 2, DIFF_QK_HALF),
            dv.reshape(B, S, N_DIFF_HEADS, DIFF_HEAD_DIM),
            lam, lam_init, subln_g[l], slopes_diff)
        dsa_out = _dsa_attention(
            sq.reshape(B, S, N_DSA_HEADS, DSA_HEAD_DIM), sk, sv,
            iq.reshape(B, S, N_IDX_HEADS, IDX_HEAD_DIM), ik, iw, slopes_dsa)
        mixed = jnp.concatenate([diff_out.reshape(B, S, DIFF_W), dsa_out.reshape(B, S, DSA_QW)], axis=-1) @ w_o[l]
        x = _layer_norm(ALPHA * x + mixed, ln1_g[l], ln1_b[l])
        y = _peer(x, peer_wq[l], peer_k1[l], peer_k2[l], peer_u[l], peer_v[l])
        x = _layer_norm(ALPHA * x + y, ln2_g[l], ln2_b[l])
    return x
```

```python
import math
from contextlib import ExitStack

import numpy as np
import concourse.bass as bass
import concourse.mybir as mybir
from concourse.bass_utils import run_bass_kernel_spmd

F32 = mybir.dt.float32
BF16 = mybir.dt.bfloat16
I32 = mybir.dt.int32
U32 = mybir.dt.uint32
AF = mybir.ActivationFunctionType
ALU = mybir.AluOpType
AX = mybir.AxisListType

D = 2048
S_LEN = 2048
NSEQ = 2
NTT = S_LEN // 128
IN_COLS = 5456
C_DQ, C_DK, C_DV, C_SQ, C_SK, C_SV, C_IQ, C_IK, C_IW = 0, 1024, 2048, 3072, 4096, 4224, 4352, 5376, 5440
ALPHA = 2.0 ** 0.25
LAM_INIT = 0.8 - 0.6 * math.exp(0.0)
LN_EPS = 1e-5
RMS_EPS = 1e-5
_sl = 2.0 ** (-8.0 * np.arange(1, 17) / 16)
SLOPES_DIFF = [float(v) for v in _sl[0::2]]
SLOPES_DSA = [float(v) for v in _sl[1::2]]
SKIP_EXP = 80.0
NEG_MASK = -1.0e30
NEG_SEL = -3.0e38
QB_DSA = 256


class Sched:
    ENGS = ('pe', 'act', 'dve', 'pool', 'sp')
    EPOCH = 20000

    def __init__(self, nc, n_dma_sems=24):
        self.nc = nc
        self.ops = []
        self.res_w = {}
        self.res_r = {}
        self.n_dma_sems = n_dma_sems

    def add(self, eng, fn, r=(), w=(), dma=False):
        idx = len(self.ops)
        deps = set()
        for x in r:
            lw = self.res_w.get(x)
            if lw is not None:
                deps.add(lw)
        for x in w:
            lw = self.res_w.get(x)
            if lw is not None:
                deps.add(lw)
            deps.update(self.res_r.get(x, ()))
        for x in r:
            lst = self.res_r.setdefault(x, [])
            if not dma:
                lst[:] = [o for o in lst if self.ops[o]['dma'] or self.ops[o]['eng'] != eng]
            lst.append(idx)
        for x in w:
            self.res_w[x] = idx
            self.res_r[x] = []
        deps.discard(idx)
        self.ops.append(dict(eng=eng, fn=fn, deps=deps, dma=dma, idx=idx))
        return idx

    def finalize(self, block, stack):
        nc = self.nc
        ops = self.ops
        for o in ops:
            o['sig'] = False
        for o in ops:
            for d in o['deps']:
                od = ops[d]
                if od['dma']:
                    continue
                if od['eng'] == o['eng'] and o['eng'] == 'pe' and not o['dma']:
                    continue
                od['sig'] = True
        cnt = {e: 0 for e in self.ENGS}
        nsem = {e: 1 for e in self.ENGS}
        for o in ops:
            if o['dma'] or not o['sig']:
                continue
            e = o['eng']
            c = cnt[e]
            o['ep'] = c // self.EPOCH
            o['val'] = c % self.EPOCH + 1
            cnt[e] = c + 1
            nsem[e] = o['ep'] + 1
        sems = {e: [stack.enter_context(nc.semaphore(f"s_{e}_{i}")) for i in range(nsem[e])]
                for e in self.ENGS}
        dsems = {}
        dstate = {}
        for o in ops:
            if not o['dma']:
                continue
            e = o['eng']
            if e not in dsems:
                dsems[e] = [stack.enter_context(nc.semaphore(f"d_{e}_{i}"))
                            for i in range(self.n_dma_sems)]
                dstate[e] = dict(n=0, vals=[0] * self.n_dma_sems, last=[None] * self.n_dma_sems)
            st = dstate[e]
            k = st['n'] % self.n_dma_sems
            st['n'] += 1
            o['dsem'] = dsems[e][k]
            o['prev_on_sem'] = st['last'][k]
            st['vals'][k] += 16
            o['dval'] = st['vals'][k]
            st['last'][k] = o['idx']
        per_eng = {e: [o for o in ops if o['eng'] == e] for e in self.ENGS}
        self.stats = {e: len(per_eng[e]) for e in self.ENGS}

        def make(e):
            def body(engine):
                waited = {}
                waited_dma = set()
                for o in per_eng[e]:
                    deps = set(o['deps'])
                    if o['dma'] and o['prev_on_sem'] is not None:
                        deps.add(o['prev_on_sem'])
                    need = {}
                    for d in sorted(deps):
                        od = ops[d]
                        if od['dma']:
                            if d in waited_dma:
                                continue
                            waited_dma.add(d)
                            engine.wait_ge(od['dsem'], od['dval'])
                            continue
                        if od['eng'] == e and e == 'pe' and not o['dma']:
                            continue
                        key = (od['eng'], od['ep'])
                        if waited.get(key, 0) >= od['val']:
                            continue
                        need[key] = max(need.get(key, 0), od['val'])
                    for key, v in need.items():
                        engine.wait_ge(sems[key[0]][key[1]], v)
                        waited[key] = v
                    ins = o['fn'](engine)
                    if o['dma']:
                        ins.then_inc(o['dsem'], 16)
                    elif o['sig']:
                        ins.then_inc(sems[e][o['ep']], 1)
                if e in dstate:
                    st = dstate[e]
                    for k in range(self.n_dma_sems):
                        if st['vals'][k] > 0:
                            engine.wait_ge(dsems[e][k], st['vals'][k])
            return body

        block.tensor(make('pe'))
        block.scalar(make('act'))
        block.vector(make('dve'))
        block.gpsimd(make('pool'))
        block.sync(make('sp'))


class _Stop(Exception):
    pass


class Arena:
    def __init__(self, nc, st, name, kb):
        self.t = st.enter_context(nc.sbuf_tensor(name, [128, kb * 256], F32))
        self.words = kb * 256
        self.off = 0

    def reset(self, off=0):
        self.off = off

    def alloc(self, dtype, *free):
        n = 1
        for f in free:
            n *= f
        esz = 4 if dtype in (F32, I32, U32) else 2
        words = (n * esz + 3) // 4
        assert self.off + words <= self.words, (self.off, words, self.words)
        ap = self.t[:, self.off:self.off + words]
        self.off += words
        if dtype != F32:
            ap = ap.bitcast(dtype)
        if len(free) == 2:
            ap = ap.rearrange("p (a b) -> p a b", a=free[0])
        elif len(free) == 3:
            ap = ap.rearrange("p (a b c) -> p a b c", a=free[0], b=free[1])
        return ap


def build_program(stage="full"):
    nc = bass.Bass("TRN2", target_bir_lowering=False)
    x = nc.dram_tensor("x", [NSEQ, S_LEN, D], F32, kind="ExternalInput").ap()
    w_in = nc.dram_tensor("w_in", [D, IN_COLS], F32, kind="ExternalInput").ap()
    w_o = nc.dram_tensor("w_o", [D, D], F32, kind="ExternalInput").ap()
    lq1 = nc.dram_tensor("lambda_q1", [1, 64], F32, kind="ExternalInput").ap()
    lk1 = nc.dram_tensor("lambda_k1", [1, 64], F32, kind="ExternalInput").ap()
    lq2 = nc.dram_tensor("lambda_q2", [1, 64], F32, kind="ExternalInput").ap()
    lk2 = nc.dram_tensor("lambda_k2", [1, 64], F32, kind="ExternalInput").ap()
    subln_g = nc.dram_tensor("subln_g", [1, 128], F32, kind="ExternalInput").ap()
    ln1_g = nc.dram_tensor("ln1_g", [1, D], F32, kind="ExternalInput").ap()
    ln1_b = nc.dram_tensor("ln1_b", [1, D], F32, kind="ExternalInput").ap()
    peer_wq = nc.dram_tensor("peer_wq", [D, D], F32, kind="ExternalInput").ap()
    peer_k1 = nc.dram_tensor("peer_k1", [128, 128], F32, kind="ExternalInput").ap()
    peer_k2 = nc.dram_tensor("peer_k2", [128, 128], F32, kind="ExternalInput").ap()
    peer_u = nc.dram_tensor("peer_u", [16384, D], F32, kind="ExternalInput").ap()
    peer_v = nc.dram_tensor("peer_v", [16384, D], F32, kind="ExternalInput").ap()
    ln2_g = nc.dram_tensor("ln2_g", [1, D], F32, kind="ExternalInput").ap()
    ln2_b = nc.dram_tensor("ln2_b", [1, D], F32, kind="ExternalInput").ap()
    out = nc.dram_tensor("out", [NSEQ * S_LEN, D], F32, kind="ExternalOutput").ap()
    if stage.startswith("Bo"):
        x1s = nc.dram_tensor("x1s", [NSEQ * S_LEN, D], F32, kind="ExternalInput").ap()
    else:
        x1s = nc.dram_tensor("x1s", [NSEQ * S_LEN, D], F32).ap()
    dbg = None
    if stage.startswith("dbg"):
        dbg = nc.dram_tensor("dbg", [128, 16 * 2048], BF16, kind="ExternalOutput").ap()
    uvbf = nc.dram_tensor("uvbf", [16384, 2 * D], BF16).ap()
    sq_d = nc.dram_tensor("sq_d", [8, 128, S_LEN], BF16).ap()
    iq_d = nc.dram_tensor("iq_d", [8, 128, S_LEN], BF16).ap()

    w_in_v = w_in.rearrange("(k p) c -> p k c", p=128)

    with ExitStack() as st:
        ar = Arena(nc, st, "arena", 207)
        ps = st.enter_context(nc.psum_tensor("ps", [128, 8, 512], F32))
        block = st.enter_context(nc.Block())
        S = Sched(nc)

        def bank(i):
            return ps[:, i, :]

        big1 = ar.alloc(BF16, 16, 2048)
        big2 = ar.alloc(BF16, 16, 2048)
        ident_f = ar.alloc(F32, 128)
        ident_b = ar.alloc(BF16, 128)
        ones_b = ar.alloc(BF16, 128)
        onesf = ar.alloc(BF16, 128)
        small = ar.alloc(F32, 16)
        workB0 = ar.off
        val = ar.alloc(F32, 512)
        valc = ar.alloc(F32, 512)
        lamv = ar.alloc(F32, 4, 64)
        lamt = ar.alloc(F32, 2, 64)
        neg_lam = small[:, 0:1]
        g08 = small[:, 1:2]
        eps_ln = small[:, 2:3]
        esum = small[:, 4:6]
        work0 = ar.off

        xT = big1
        catT = big2

        S.add('pool', lambda e: e.memset(ident_f, 0.0), w=['ident_f'])
        S.add('pool', lambda e: e.affine_select(out=ident_f, in_=ident_f, pattern=[[-1, 128]],
                                                 compare_op=ALU.not_equal, fill=1.0, base=0,
                                                 channel_multiplier=1), r=['ident_f'], w=['ident_f'])
        S.add('pool', lambda e: e.tensor_copy(out=ident_b, in_=ident_f), r=['ident_f'], w=['ident_b'])
        S.add('pool', lambda e: e.memset(ones_b, 1.0), w=['ones_b'])
        S.add('pool', lambda e: e.memset(onesf, 1.0 / 128.0), w=['onesf'])
        S.add('pool', lambda e: e.memset(eps_ln, LN_EPS), w=['eps'])
        S.add('pool', lambda e: e.iota(val, [[1, 512]], base=0, channel_multiplier=-1,
                                       allow_small_or_imprecise_dtypes=True), w=['val'])
        S.add('pool', lambda e: e.tensor_scalar(out=valc, in0=val, scalar1=0.0, scalar2=None, op0=ALU.max),
              r=['val'], w=['valc'])
        for i, t in enumerate((lq1, lk1, lq2, lk2)):
            S.add('sp', lambda e, i=i, t=t: e.dma_start(out=lamv[:, i, :], in_=t[0:1, :].to_broadcast([128, 64])),
                  w=[('lamv', i)], dma=True)
        S.add('sp', lambda e: e.dma_start(out=g08, in_=subln_g.rearrange("o (p u) -> (o p) u", u=1)),
              w=['g08'], dma=True)
        for i in range(2):
            S.add('dve', lambda e, i=i: e.tensor_tensor(out=lamt[:, i, :], in0=lamv[:, 2 * i, :],
                                                        in1=lamv[:, 2 * i + 1, :], op=ALU.mult),
                  r=[('lamv', 2 * i), ('lamv', 2 * i + 1)], w=[('lamt', i)])
            S.add('dve', lambda e, i=i: e.reduce_sum(out=esum[:, i:i + 1], in_=lamt[:, i, :], axis=AX.X),
                  r=[('lamt', i)], w=[('esum', i)])
        S.add('act', lambda e: e.activation(out=esum, in_=esum, func=AF.Exp),
              r=[('esum', 0), ('esum', 1)], w=['esum_e'])
        S.add('dve', lambda e: e.tensor_tensor(out=neg_lam, in0=esum[:, 1:2], in1=esum[:, 0:1], op=ALU.subtract),
              r=['esum_e'], w=['neg_lam'])
        S.add('dve', lambda e: e.tensor_scalar(out=neg_lam, in0=neg_lam, scalar1=-LAM_INIT, scalar2=None, op0=ALU.add),
              r=['neg_lam'], w=['neg_lam'])
        S.add('dve', lambda e: e.tensor_scalar(out=g08, in0=g08, scalar1=1.0 - LAM_INIT, scalar2=None, op0=ALU.mult),
              r=['g08'], w=['g08'])

        rr = {'cp': 0}

        def evac(out_ap, in_ap, r, w):
            rr['cp'] += 1
            if rr['cp'] % 2:
                S.add('act', lambda e: e.activation(out=out_ap, in_=in_ap, func=AF.Copy), r=r, w=w)
            else:
                S.add('dve', lambda e: e.tensor_copy(out=out_ap, in_=in_ap), r=r, w=w)

        def mm(out_ap, lhsT, rhs, start, stop, r, w):
            S.add('pe', lambda e: e.matmul(out_ap, lhsT=lhsT, rhs=rhs, start=start, stop=stop), r=r, w=w)

        wch_n = {'i': 0}
        TCH = 1024
        tbl_chunks = [(src, c0, r0) for (src, c0) in ((peer_u, 0), (peer_v, D))
                      for r0 in range(0, 16384, TCH)]
        tbl_state = {'i': 0}

        def emit_tbl_chunk():
            if not (stage == "full" or stage.startswith("B")) or tbl_state['i'] >= len(tbl_chunks):
                tbl_state['i'] = max(tbl_state['i'], len(tbl_chunks)) if not (stage == "full" or stage.startswith("B")) else tbl_state['i']
                return
            src, c0, r0 = tbl_chunks[tbl_state['i']]
            key = ('tbl', tbl_state['i'])
            tbl_state['i'] += 1
            S.add('pool', lambda e: e.dma_start(out=uvbf[r0:r0 + TCH, c0:c0 + D], in_=src[r0:r0 + TCH, :]),
                  w=[key], dma=True)

        def phase_a(b):
            gate = [('x1s', b - 1, tt) for tt in range(NTT)] if b > 0 else []
            ar.reset(work0)
            xs = [ar.alloc(F32, 2048) for _ in range(2)]
            a1_end = ar.off
            for tt in range(NTT):
                xb = xs[tt % 2]
                S.add('sp', lambda e, xb=xb, tt=tt: e.dma_start(out=xb, in_=x[b, tt * 128:(tt + 1) * 128, :]),
                      w=[('xs', tt % 2)], dma=True)
                for g in range(4):
                    pb = 6 + g % 2
                    for kk in range(4):
                        k = g * 4 + kk
                        S.add('pe', lambda e, pb=pb, kk=kk, k=k, xb=xb: e.transpose(
                            out=bank(pb)[:, kk * 128:(kk + 1) * 128], in_=xb[:, k * 128:(k + 1) * 128],
                            identity=ident_f), r=[('xs', tt % 2), 'ident_f'], w=[('ps', pb)])
                    evac(xT[:, g * 4:(g + 1) * 4, tt * 128:(tt + 1) * 128],
                         bank(pb).rearrange("p (a c) -> p a c", a=4), r=[('ps', pb)], w=[('xT', tt)])
            xT_all = [('xT', tt) for tt in range(NTT)]
            gate = gate + xT_all
            if stage == "dbgA1":
                S.add('sp', lambda e: e.dma_start(out=dbg, in_=big1.rearrange("p a b -> p (a b)")), r=xT_all, dma=True)
                raise _Stop()

            ar.reset(work0)
            wch = [ar.alloc(BF16, 16, 128) for _ in range(3)]
            tdiag = [ar.alloc(BF16, 512) for _ in range(2)]
            toff = [ar.alloc(BF16, 512) for _ in range(2)]
            Eb = [ar.alloc(BF16, 512) for _ in range(2)]
            Pb = [ar.alloc(BF16, 512) for _ in range(2)]
            lnz = [ar.alloc(F32, 512)]
            att0 = ar.off
            lnz.append(ar.alloc(F32, 512))
            fA = ar.alloc(F32, 512)
            fB = ar.alloc(F32, 512)
            fD = ar.alloc(F32, 512)
            fS = ar.alloc(BF16, 512)
            fR = ar.alloc(F32, 512)

            def load_w(c0, ncols=128, dst_col=0, slot=None, new=True, slots=(0, 1, 2)):
                if new:
                    wch_n['i'] += 1
                sl = slots[wch_n['i'] % len(slots)]
                dst = wch[sl]
                S.add('pool', lambda e: e.dma_start(out=dst[:, :, dst_col:dst_col + ncols],
                                                    in_=w_in_v[:, :, c0:c0 + ncols]),
                      r=gate, w=[('wch', sl)], dma=True)
                if b == NSEQ - 1 and new:
                    emit_tbl_chunk()
                return sl

            def proj_fm(sl, dst, ncols_t, t0, r_extra=(), wkey=None, m=128, banks=(6, 7)):
                nb = (ncols_t + 511) // 512
                for tb in range(nb):
                    n = min(512, ncols_t - tb * 512)
                    pb = banks[rr['cp'] % 2]
                    for k in range(16):
                        mm(bank(pb)[:m, :n], wch[sl][:, k, :m], xT[:, k, t0 + tb * 512:t0 + tb * 512 + n],
                           k == 0, k == 15, r=[('wch', sl)] + xT_all, w=[('ps', pb)])
                    evac(dst[:m, tb * 512:tb * 512 + n], bank(pb)[:m, :n], r=[('ps', pb)], w=[wkey])

            def proj_tm(sl, dst, ncols, wkey):
                per = 512 // ncols
                per = min(per, 16)
                for g in range(16 // per):
                    pb = 6 + rr['cp'] % 2
                    for j in range(per):
                        tt = g * per + j
                        for k in range(16):
                            mm(bank(pb)[:, j * ncols:(j + 1) * ncols], xT[:, k, tt * 128:(tt + 1) * 128],
                               wch[sl][:, k, :ncols], k == 0, k == 15, r=[('wch', sl)] + xT_all, w=[('ps', pb)])
                    evac(dst[:, g * per:(g + 1) * per, :],
                         bank(pb)[:, :per * ncols].rearrange("p (a c) -> p a c", a=per),
                         r=[('ps', pb)], w=[wkey])

            def make_T(slope, par):
                S.add('act', lambda e: e.activation(out=tdiag[par], in_=valc, func=AF.Exp, scale=-slope),
                      r=['valc'] + gate, w=[('tdiag', par)])
                S.add('pool', lambda e: e.affine_select(out=tdiag[par], in_=tdiag[par], pattern=[[1, 512]],
                                                        compare_op=ALU.is_ge, fill=0.0, base=0,
                                                        channel_multiplier=-1),
                      r=[('tdiag', par)], w=[('tdiag', par)])
                S.add('act', lambda e: e.activation(out=toff[par], in_=val, func=AF.Exp, scale=-slope,
                                                    bias=-slope * 128.0),
                      r=['val'] + gate, w=[('toff', par)])

            def attn_block(slope, par, q0, QB, kq_list, v_of, sel_of, scale, o_banks, z_banks, n_c, hook=None):
                first_tile = q0 // 128
                tiles = []
                for stile in range(0, first_tile + QB // 128):
                    if stile < first_tile:
                        r_ = first_tile - stile
                        if slope * 128.0 * (r_ - 1) > SKIP_EXP:
                            continue
                    tiles.append(stile)
                items = [(c, stile) for c in range(n_c) for stile in tiles]
                pend = None
                for i, (c, stile) in enumerate(items):
                    sb = i % 2
                    j = stile - first_tile
                    col0 = 128 * j if j > 0 else 0
                    n = QB - col0
                    kT_ap, qT_ap, rk = kq_list[c](stile, col0, n)
                    mm(bank(sb)[:, :n], kT_ap, qT_ap, True, True, r=rk, w=[('ps', sb)])
                    if j >= 0:
                        bias = 0.0
                        tm = tdiag[par]
                        tkey = ('tdiag', par)
                    else:
                        bias = -slope * 128.0 * (-j - 1)
                        tm = toff[par]
                        tkey = ('toff', par)
                    S.add('act', lambda e, sb=sb, n=n, bias=bias: e.activation(
                        out=Eb[sb][:, :n], in_=bank(sb)[:, :n], func=AF.Exp, scale=scale, bias=bias),
                        r=[('ps', sb)], w=[('E', sb)])
                    S.add('dve', lambda e, sb=sb, n=n, tm=tm: e.tensor_tensor(
                        out=Pb[sb][:, :n], in0=Eb[sb][:, :n], in1=tm[:, :n], op=ALU.mult),
                        r=[('E', sb), tkey], w=[('P', sb)])
                    if sel_of is not None:
                        sel_ap, sel_key = sel_of(stile, col0, n)
                        S.add('pool', lambda e, sb=sb, n=n, sel_ap=sel_ap: e.tensor_tensor(
                            out=Pb[sb][:, :n], in0=Pb[sb][:, :n], in1=sel_ap, op=ALU.mult),
                            r=[('P', sb), sel_key], w=[('P', sb)])
                    if pend is not None:
                        pend()
                    first = (stile == tiles[0])
                    last = (stile == tiles[-1])
                    v_ap, v_key = v_of(stile)

                    def pv(c=c, sb=sb, col0=col0, n=n, first=first, last=last, v_ap=v_ap, v_key=v_key):
                        mm(bank(o_banks[c])[:, col0:col0 + n], v_ap, Pb[sb][:, :n], first, last,
                           r=[('P', sb), v_key], w=[('ps', o_banks[c])])
                        mm(bank(z_banks[c])[:, col0:col0 + n], ones_b, Pb[sb][:, :n], first, last,
                           r=[('P', sb), 'ones_b'], w=[('ps', z_banks[c])])
                    pend = pv
                    if hook is not None:
                        hook()
                pend()

            def recip_z(c, zb, QB):
                S.add('act', lambda e: e.activation(out=lnz[c][:, :QB], in_=bank(zb)[:, :QB], func=AF.Ln),
                      r=[('ps', zb)], w=[('lnz', c)])
                S.add('act', lambda e: e.activation(out=lnz[c][:, :QB], in_=lnz[c][:, :QB], func=AF.Exp, scale=-1.0),
                      r=[('lnz', c)], w=[('lnz', c)])

            qT_h = ar.alloc(BF16, 2048)
            kT_h = ar.alloc(BF16, 2048)
            v_h = ar.alloc(BF16, 16, 128)
            scale_d = 64 ** -0.5
            wch.extend(ar.alloc(BF16, 16, 128) for _ in range(3))
            S6 = (0, 1, 2, 3, 4, 5)
            nxt = [load_w(C_DQ, slots=S6), load_w(C_DK, slots=S6), load_w(C_DV, slots=S6)]
            for h in range(8):
                par = h % 2
                slope = SLOPES_DIFF[h]
                make_T(slope, par)
                slq, slk, slv = nxt
                proj_fm(slq, qT_h, 2048, 0, wkey='qT_h')
                proj_fm(slk, kT_h, 2048, 0, wkey='kT_h')
                proj_tm(slv, v_h, 128, wkey='v_h')
                if h + 1 < 8:
                    nxt = [load_w(C_DQ + (h + 1) * 128, slots=S6), load_w(C_DK + (h + 1) * 128, slots=S6),
                           load_w(C_DV + (h + 1) * 128, slots=S6)]
                if stage == "dbgP":
                    S.add('sp', lambda e: e.dma_start(out=dbg[:, 0:2048], in_=qT_h), r=['qT_h'], dma=True)
                    S.add('sp', lambda e: e.dma_start(out=dbg[:, 2048:4096], in_=kT_h), r=['kT_h'], dma=True)
                    S.add('sp', lambda e: e.dma_start(out=dbg[:, 4096:6144], in_=v_h.rearrange("p a b -> p (a b)")), r=['v_h'], dma=True)
                    S.add('sp', lambda e: e.dma_start(out=dbg[:, 6144:6656], in_=tdiag[par]), r=[('tdiag', par)], dma=True)
                    S.add('sp', lambda e: e.dma_start(out=dbg[:, 6656:7168], in_=toff[par]), r=[('toff', par)], dma=True)
                    raise _Stop()
                for qb in range(4):
                    q0 = qb * 512

                    def kq(c):
                        def f(stile, col0, n):
                            return (kT_h[c * 64:(c + 1) * 64, stile * 128:(stile + 1) * 128],
                                    qT_h[c * 64:(c + 1) * 64, q0 + col0:q0 + col0 + n],
                                    ['qT_h', 'kT_h'])
                        return f
                    attn_block(slope, par, q0, 512, [kq(0), kq(1)],
                               lambda stile: (v_h[:, stile, :], 'v_h'), None, scale_d,
                               [2, 3], [4, 5], 2)
                    recip_z(0, 4, 512)
                    recip_z(1, 5, 512)
                    S.add('dve', lambda e: e.tensor_tensor(out=fA, in0=bank(2), in1=lnz[0], op=ALU.mult),
                          r=[('ps', 2), ('lnz', 0)], w=['fA'])
                    S.add('dve', lambda e: e.tensor_tensor(out=fB, in0=bank(3), in1=lnz[1], op=ALU.mult),
                          r=[('ps', 3), ('lnz', 1)], w=['fB'])
                    S.add('dve', lambda e: e.scalar_tensor_tensor(out=fD, in0=fB, scalar=neg_lam, in1=fA,
                                                                  op0=ALU.mult, op1=ALU.add),
                          r=['fA', 'fB', 'neg_lam'], w=['fD'])
                    S.add('pool', lambda e: e.tensor_tensor(out=fS, in0=fD, in1=fD, op=ALU.mult),
                          r=['fD'], w=['fS'])
                    pbq = 6 + rr['cp'] % 2
                    rr['cp'] += 1
                    mm(bank(pbq), onesf, fS, True, True, r=['onesf', 'fS'], w=[('ps', pbq)])
                    S.add('act', lambda e, pbq=pbq: e.activation(out=fR, in_=bank(pbq), func=AF.Ln, bias=eps_ln),
                          r=[('ps', pbq), 'eps'], w=['fR'])
                    S.add('act', lambda e: e.activation(out=fR, in_=fR, func=AF.Exp, scale=-0.5),
                          r=['fR'], w=['fR'])
                    S.add('dve', lambda e, h=h, q0=q0: e.scalar_tensor_tensor(
                        out=catT[:, h, q0:q0 + 512], in0=fD, scalar=g08, in1=fR, op0=ALU.mult, op1=ALU.mult),
                        r=['fD', 'fR', 'g08'], w=[('catT', h)])
                if stage == "dbgA2" and h == 0:
                    S.add('sp', lambda e: e.dma_start(out=dbg[:, 0:2048], in_=big2[:, 0, :]), r=[('catT', 0)], dma=True)
                    raise _Stop()
            if stage == "dbgA2all":
                S.add('sp', lambda e: e.dma_start(out=dbg[:, 0:8 * 2048], in_=big2[:, 0:8, :].rearrange("p a b -> p (a b)")), r=[('catT', hh) for hh in range(8)], dma=True)
                raise _Stop()

            ar.reset(att0)
            skT = ar.alloc(BF16, 2048)
            ikT2 = ar.alloc(BF16, 2048)
            sv = ar.alloc(BF16, 16, 128)
            iwa = ar.alloc(F32, 16, 16)
            iws = ar.alloc(F32, 16, 16)
            selT = ar.alloc(BF16, 16, QB_DSA)
            isc = ar.alloc(F32, 2048)
            selb = [ar.alloc(BF16, 2048) for _ in range(QB_DSA // 128)]
            iqT = ar.alloc(BF16, 8, QB_DSA)
            sq_blk = ar.alloc(BF16, 8, QB_DSA)
            rbf = [ar.alloc(BF16, 512) for _ in range(2)]
            sgd = ar.alloc(BF16, 4, 128)
            m8 = ar.alloc(F32, 8)
            bs = m8
            NBIS = 24

            LIVE = (0, 1)
            BG = (2,)
            sl = load_w(C_SK)
            proj_fm(sl, skT, 2048, 0, wkey='skT')
            sl = load_w(C_IK, 64, 0)
            load_w(C_IK, 64, 64, new=False)
            proj_fm(sl, ikT2, 2048, 0, wkey='ikT2')
            sl = load_w(C_SV)
            proj_tm(sl, sv, 128, wkey='sv')
            sl = load_w(C_IW, 16, 0)
            iw = isc.rearrange("p (a c) -> p a c", a=16)[:, :, 0:16]
            proj_tm(sl, iw, 16, wkey=('isc', 0))
            S.add('dve', lambda e: e.tensor_scalar(out=iws, in0=iw, scalar1=0.0, scalar2=None, op0=ALU.is_ge),
                  r=[('isc', 0)], w=['iws'])
            S.add('dve', lambda e: e.tensor_scalar(out=iws, in0=iws, scalar1=2.0, scalar2=-1.0, op0=ALU.mult, op1=ALU.add),
                  r=['iws'], w=['iws'])
            S.add('dve', lambda e: e.tensor_tensor(out=iwa, in0=iw, in1=iws, op=ALU.mult),
                  r=[('isc', 0), 'iws'], w=['iwa'])
            scale_s = 128 ** -0.5
            NQT = QB_DSA // 128
            NQB = S_LEN // QB_DSA
            isc_all = [('isc', ch) for ch in range(4)]
            for i_ in range(16):
                tmpT = selb[i_ % 2]
                c0_ = (C_SQ if i_ < 8 else C_IQ) + (i_ % 8) * 128
                dst_ = (sq_d if i_ < 8 else iq_d)[i_ % 8]
                sl = load_w(c0_)
                proj_fm(sl, tmpT, 2048, 0, wkey=('selb', i_ % 2))
                S.add('sp', lambda e, dst_=dst_, tmpT=tmpT: e.dma_start(out=dst_, in_=tmpT),
                      r=[('selb', i_ % 2)], w=[('qd', i_)], dma=True)
            qd_sq = [('qd', i_) for i_ in range(8)]
            qd_iq = [('qd', 8 + i_) for i_ in range(8)]
            wo_b = big1
            for kq4 in range(4):
                S.add('pool', lambda e, kq4=kq4: e.dma_start(
                    out=wo_b[:, kq4 * 4:(kq4 + 1) * 4, :],
                    in_=w_o.rearrange("(k p) c -> p k c", p=128)[:, kq4 * 4:(kq4 + 1) * 4, :]),
                    w=xT_all + ['wo_b'], dma=True)

            def bg_block(qb):
                q0 = qb * QB_DSA
                S.add('sp', lambda e: e.dma_start(out=iqT, in_=iq_d[:, :, q0:q0 + QB_DSA].rearrange("h p t -> p h t")),
                      r=qd_iq, w=[('iqT', pair) for pair in range(8)], dma=True)
                for ql in range(NQT):
                    qt = qb * NQT + ql
                    nk = (qt + 1) * 128
                    nch = (nk + 511) // 512
                    for ch in range(nch):
                        n = min(512, nk - ch * 512)
                        for h in range(16):
                            pair, half = h // 2, h % 2
                            pbk = (3, 7)[h % 2]
                            mm(bank(pbk)[:, :n], iqT[half * 64:(half + 1) * 64, pair, ql * 128:(ql + 1) * 128],
                               ikT2[half * 64:(half + 1) * 64, ch * 512:ch * 512 + n], True, True,
                               r=[('iqT', pair), 'ikT2'], w=[('ps', pbk)])
                            rt = rbf[h % 2]
                            rk = ('rbf', h % 2)
                            S.add('act', lambda e, pbk=pbk, n=n, rt=rt, qt=qt, h=h: e.activation(
                                out=rt[:, :n], in_=bank(pbk)[:, :n], func=AF.Relu, scale=iwa[:, qt, h:h + 1]),
                                r=[('ps', pbk), 'iwa'], w=[rk])
                            S.add('pool', lambda e, h=h, qt=qt: e.tensor_scalar(
                                out=sgd[:, h % 4, :], in0=ident_b, scalar1=iws[:, qt, h:h + 1], scalar2=None, op0=ALU.mult),
                                r=['ident_b', 'iws'], w=[('sgd', h % 4)])
                            mm(bank(5)[:, :n], sgd[:, h % 4, :], rt[:, :n], h == 0, h == 15,
                               r=[rk, ('sgd', h % 4)], w=[('ps', 5)])
                        S.add('act', lambda e, n=n, ch=ch: e.activation(out=isc[:, ch * 512:ch * 512 + n], in_=bank(5)[:, :n],
                                                                        func=AF.Copy),
                              r=[('ps', 5)], w=[('isc', ch)])
                    sb_ = selb[ql]
                    if qt >= 2:
                        S.add('dve', lambda e, nk=nk: e.tensor_reduce(out=bs[:, 0:1], in_=isc[:, :nk], axis=AX.X, op=ALU.max,
                                                                      apply_absolute_value=True),
                              r=isc_all, w=['bs_c'])
                        S.add('dve', lambda e: e.tensor_scalar(out=bs[:, 1:2], in0=bs[:, 0:1], scalar1=2.0, scalar2=2.0,
                                                               op0=ALU.mult, op1=ALU.add), r=['bs_c'], w=['bs_w'])
                        S.add('dve', lambda e: e.tensor_scalar(out=bs[:, 2:3], in0=bs[:, 0:1], scalar1=-1.0, scalar2=-1.0,
                                                               op0=ALU.mult, op1=ALU.add), r=['bs_c'], w=['bs_lo'])
                    S.add('pool', lambda e, qt=qt: e.affine_select(
                        out=isc[:, qt * 128:(qt + 1) * 128], in_=isc[:, qt * 128:(qt + 1) * 128],
                        pattern=[[-1, 128]], compare_op=ALU.is_ge, fill=NEG_MASK, base=0, channel_multiplier=1),
                        r=isc_all + ['bs_c'], w=isc_all)
                    if qt >= 2:
                        for it in range(NBIS):
                            ck = 0.5 ** (it + 1)
                            S.add('dve', lambda e, ck=ck: e.scalar_tensor_tensor(
                                out=bs[:, 3:4], in0=bs[:, 1:2], scalar=ck, in1=bs[:, 2:3], op0=ALU.mult, op1=ALU.add),
                                r=['bs_w', 'bs_lo'], w=['bs_mid'])
                            S.add('dve', lambda e, nk=nk, sb_=sb_: e.tensor_scalar(
                                out=sb_[:, :nk], in0=isc[:, :nk], scalar1=bs[:, 3:4], scalar2=0.0, op0=ALU.is_ge,
                                op1=ALU.add, accum_out=bs[:, 4:5]),
                                r=isc_all + ['bs_mid'], w=[('selb', ql), 'bs_cnt'])
                            S.add('dve', lambda e: e.tensor_scalar(
                                out=bs[:, 5:6], in0=bs[:, 4:5], scalar1=255.5, scalar2=bs[:, 1:2], op0=ALU.is_ge,
                                op1=ALU.mult), r=['bs_cnt', 'bs_w'], w=['bs_g'])
                            S.add('dve', lambda e, ck=ck: e.scalar_tensor_tensor(
                                out=bs[:, 2:3], in0=bs[:, 5:6], scalar=ck, in1=bs[:, 2:3], op0=ALU.mult, op1=ALU.add),
                                r=['bs_g', 'bs_lo'], w=['bs_lo'])
                        S.add('dve', lambda e, nk=nk, sb_=sb_: e.tensor_scalar(out=sb_[:, :nk], in0=isc[:, :nk],
                                                                               scalar1=bs[:, 2:3], scalar2=None, op0=ALU.is_ge),
                              r=isc_all + ['bs_lo'], w=[('selb', ql)])
                    else:
                        S.add('dve', lambda e, nk=nk, sb_=sb_: e.tensor_scalar(out=sb_[:, :nk], in0=isc[:, :nk], scalar1=-1.0e29,
                                                                               scalar2=None, op0=ALU.is_ge),
                              r=isc_all, w=[('selb', ql)])

            def build_selT(qb):
                for ql in range(NQT):
                    qt = qb * NQT + ql
                    sb_ = selb[ql]
                    for g in range((qt + 4) // 4):
                        pb = 6
                        pbv = bank(pb).bitcast(BF16)
                        cnt = min(4, qt + 1 - g * 4)
                        for kk in range(cnt):
                            stile = g * 4 + kk
                            S.add('pe', lambda e, pbv=pbv, kk=kk, stile=stile, sb_=sb_: e.transpose(
                                out=pbv[:, kk * 128:(kk + 1) * 128], in_=sb_[:, stile * 128:(stile + 1) * 128],
                                identity=ident_b), r=[('selb', ql), 'ident_b'], w=[('ps', pb)])
                        evac(selT[:, g * 4:g * 4 + cnt, ql * 128:(ql + 1) * 128],
                             pbv[:, :cnt * 128].rearrange("p (a c) -> p a c", a=cnt),
                             r=[('ps', pb)], w=['selT'])

            def record(fn):
                rec = []
                orig = S.add
                S.add = lambda *a_, **k_: rec.append((a_, k_))
                try:
                    fn()
                finally:
                    S.add = orig
                return rec

            bg_block(0)
            for qb in range(NQB):
                q0 = qb * QB_DSA
                build_selT(qb)
                rec = record(lambda: bg_block(qb + 1)) if qb + 1 < NQB else []
                n_items = 0
                first_tile = q0 // 128
                for h in range(8):
                    for stile in range(0, first_tile + NQT):
                        if stile < first_tile and SLOPES_DSA[h] * 128.0 * (first_tile - stile - 1) > SKIP_EXP:
                            continue
                        n_items += 1
                per = (len(rec) + n_items - 1) // n_items if rec else 0
                rstate = {'i': 0}

                def hook():
                    for _ in range(per):
                        if rstate['i'] < len(rec):
                            a_, k_ = rec[rstate['i']]
                            S.add(*a_, **k_)
                            rstate['i'] += 1
                S.add('sp', lambda e, q0=q0: e.dma_start(out=sq_blk, in_=sq_d[:, :, q0:q0 + QB_DSA].rearrange("h p t -> p h t")),
                      r=qd_sq, w=['sq_blk'], dma=True)
                for h in range(8):
                    par = h % 2
                    slope = SLOPES_DSA[h]
                    make_T(slope, par)
                    sq_cur = sq_blk[:, h, :]

                    def kq(stile, col0, n, sq_cur=sq_cur, h=h):
                        return (skT[:, stile * 128:(stile + 1) * 128], sq_cur[:, col0:col0 + n],
                                ['skT', 'sq_blk'])
                    attn_block(slope, par, q0, QB_DSA, [kq],
                               lambda stile: (sv[:, stile, :], 'sv'),
                               lambda stile, col0, n: (selT[:, stile, col0:col0 + n], 'selT'),
                               scale_s, [2], [4], 1, hook=hook)
                    recip_z(0, 4, QB_DSA)
                    S.add('dve', lambda e, h=h, q0=q0: e.tensor_tensor(
                        out=catT[:, 8 + h, q0:q0 + QB_DSA], in0=bank(2)[:, :QB_DSA], in1=lnz[0][:, :QB_DSA],
                        op=ALU.mult), r=[('ps', 2), ('lnz', 0)], w=[('catT', 8 + h)])
                while rstate['i'] < len(rec):
                    a_, k_ = rec[rstate['i']]
                    S.add(*a_, **k_)
                    rstate['i'] += 1

            if stage == "dbgA3":
                S.add('sp', lambda e: e.dma_start(out=dbg, in_=big2.rearrange("p a b -> p (a b)")), r=[('catT', hh) for hh in range(16)], dma=True)
                raise _Stop()
            ar.reset(work0)
            xs = [ar.alloc(F32, 2048) for _ in range(2)]
            lng = ar.alloc(F32, 2048)
            lnb = ar.alloc(F32, 2048)
            rb = [ar.alloc(F32, 2048) for _ in range(2)]
            st6 = ar.alloc(F32, 4, 6)
            mv = ar.alloc(F32, 2)
            rstd = ar.alloc(F32, 1)
            wo_b = big1
            cat_all = [('catT', hh) for hh in range(16)]
            S.add('sp', lambda e: e.dma_start(out=lng, in_=ln1_g[0:1, :].to_broadcast([128, 2048])),
                  r=cat_all, w=['lng'], dma=True)
            S.add('sp', lambda e: e.dma_start(out=lnb, in_=ln1_b[0:1, :].to_broadcast([128, 2048])),
                  r=cat_all, w=['lnb'], dma=True)
            for tt in range(NTT):
                xb = xs[tt % 2]
                rbuf = rb[tt % 2]
                S.add('sp', lambda e, xb=xb, tt=tt: e.dma_start(out=xb, in_=x[b, tt * 128:(tt + 1) * 128, :]),
                      r=cat_all, w=[('xs', tt % 2)], dma=True)
                for cb in range(4):
                    pbo = 4 * (tt % 2) + cb
                    for k in range(16):
                        mm(bank(pbo), catT[:, k, tt * 128:(tt + 1) * 128], wo_b[:, k, cb * 512:(cb + 1) * 512],
                           k == 0, k == 15, r=cat_all + ['wo_b'] + xT_all, w=[('ps', pbo)])
                    S.add('dve', lambda e, cb=cb, xb=xb, rbuf=rbuf, pbo=pbo: e.scalar_tensor_tensor(
                        out=rbuf[:, cb * 512:(cb + 1) * 512], in0=xb[:, cb * 512:(cb + 1) * 512], scalar=ALPHA,
                        in1=bank(pbo), op0=ALU.mult, op1=ALU.add),
                        r=[('xs', tt % 2), ('ps', pbo)], w=[('rb', tt % 2, cb)])
                    S.add('dve', lambda e, cb=cb, rbuf=rbuf: e.bn_stats(out=st6[:, cb, :], in_=rbuf[:, cb * 512:(cb + 1) * 512]),
                          r=[('rb', tt % 2, cb)], w=[('st6', cb)])
                rb_all = [('rb', tt % 2, cb) for cb in range(4)]
                S.add('dve', lambda e: e.bn_aggr(out=mv, in_=st6), r=[('st6', cb) for cb in range(4)], w=['mv'])
                S.add('act', lambda e: e.activation(out=rstd, in_=mv[:, 1:2], func=AF.Sqrt, bias=eps_ln),
                      r=['mv', 'eps'], w=['rstd'])
                S.add('dve', lambda e: e.reciprocal(out=rstd, in_=rstd), r=['rstd'], w=['rstd'])
                S.add('dve', lambda e, rbuf=rbuf: e.tensor_scalar(out=rbuf, in0=rbuf, scalar1=mv[:, 0:1], scalar2=rstd,
                                                                   op0=ALU.subtract, op1=ALU.mult),
                      r=rb_all + ['mv', 'rstd'], w=rb_all)
                S.add('pool', lambda e, rbuf=rbuf: e.tensor_tensor(out=rbuf, in0=rbuf, in1=lng, op=ALU.mult),
                      r=rb_all + ['lng'], w=rb_all)
                S.add('pool', lambda e, rbuf=rbuf: e.tensor_tensor(out=rbuf, in0=rbuf, in1=lnb, op=ALU.add),
                      r=rb_all + ['lnb'], w=rb_all)
                row0 = b * S_LEN + tt * 128
                dst = out if stage == "A" else x1s
                S.add('sp', lambda e, rbuf=rbuf, row0=row0, dst=dst: e.dma_start(out=dst[row0:row0 + 128, :], in_=rbuf),
                      r=rb_all, w=[('x1s', b, tt)], dma=True)


        def phase_b():
            NG = NSEQ * NTT
            gateB = [('x1s', bb, tt) for bb in range(NSEQ) for tt in range(NTT)]
            while tbl_state['i'] < len(tbl_chunks):
                emit_tbl_chunk()
            tbl_all = [('tbl', i) for i in range(len(tbl_chunks))]
            xT_all = [('xT', tt) for tt in range(NTT)]
            cat_all = [('catT', hh) for hh in range(16)]
            ar.reset(workB0)
            xs1 = ar.alloc(F32, 2048)
            x1b = [ar.alloc(BF16, 2048) for _ in range(2)]
            off_xq = ar.off
            x1T = ar.alloc(BF16, 16, 128)
            qT = ar.alloc(BF16, 16, 128)
            cand = ar.t[:, off_xq:off_xq + 2048].rearrange("p (h c) -> p h c", h=8)
            s_sb = ar.alloc(F32, 16, 128)
            lng = ar.alloc(F32, 2048)
            lnb = ar.alloc(F32, 2048)
            rb = ar.alloc(F32, 2048)
            kT = [ar.alloc(BF16, 128) for _ in range(2)]
            m16 = ar.alloc(F32, 16, 16)
            i16 = ar.alloc(U32, 16, 16)
            i16f = ar.alloc(F32, 16, 16)
            ts = ar.alloc(F32, 8, 16)
            pos = ar.alloc(U32, 8, 16)
            pa = ar.alloc(U32, 128)
            pb_ = ar.alloc(U32, 128)
            paf = ar.alloc(F32, 128)
            pbf = ar.alloc(F32, 128)
            red1 = ar.alloc(F32, 128)
            red2 = ar.alloc(F32, 128)
            iota16 = ar.alloc(F32, 16)
            eidx = [ar.alloc(I32, 128) for _ in range(2)]
            gts = [ar.alloc(F32, 8, 16) for _ in range(2)]
            negm = ar.alloc(F32, 8)
            zs = ar.alloc(F32, 8)
            hv = ar.alloc(F32, 128)
            av = ar.alloc(F32, 128)
            dg = [ar.alloc(BF16, 128) for _ in range(4)]
            st6 = ar.alloc(F32, 4, 6)
            mv = ar.alloc(F32, 2)
            rstd = ar.alloc(F32, 1)
            wq_b = big1
            ring = big2.rearrange("p (s two) d -> p s (two d)", two=2)

            for kq4 in range(4):
                S.add('pool', lambda e, kq4=kq4: e.dma_start(
                    out=wq_b[:, kq4 * 4:(kq4 + 1) * 4, :],
                    in_=peer_wq.rearrange("(k p) c -> p k c", p=128)[:, kq4 * 4:(kq4 + 1) * 4, :]),
                    r=gateB, w=xT_all + ['wo_b'], dma=True)
            S.add('sp', lambda e: e.dma_start(out=lng, in_=ln2_g[0:1, :].to_broadcast([128, 2048])),
                  r=gateB, w=['lng'], dma=True)
            S.add('sp', lambda e: e.dma_start(out=lnb, in_=ln2_b[0:1, :].to_broadcast([128, 2048])),
                  r=gateB, w=['lnb'], dma=True)
            S.add('pool', lambda e: e.iota(iota16, [[1, 16]], base=0, channel_multiplier=0,
                                           allow_small_or_imprecise_dtypes=True), r=gateB, w=['iota16'])
            for i, kk in enumerate((peer_k1, peer_k2)):
                S.add('sp', lambda e, kk=kk: e.dma_start(out=xs1[:, 0:128], in_=kk[:, :]),
                      r=gateB, w=['xs1', 'val', 'valc'] + [('lamv', q_) for q_ in range(4)] + [('lamt', q_) for q_ in range(2)],
                      dma=True)
                S.add('pe', lambda e: e.transpose(out=bank(6)[:, 0:128], in_=xs1[:, 0:128], identity=ident_f),
                      r=['xs1', 'ident_f'], w=[('ps', 6)])
                S.add('dve', lambda e, i=i: e.tensor_copy(out=kT[i], in_=bank(6)[:, 0:128]),
                      r=[('ps', 6)], w=[('kT', i)])

            def stage1(g):
                b_, tt = g // NTT, g % NTT
                p2 = g % 2
                S.add('sp', lambda e: e.dma_start(out=xs1, in_=x1s[g * 128:(g + 1) * 128, :]),
                      r=[('x1s', b_, tt)], w=['xs1'], dma=True)
                S.add('act', lambda e: e.activation(out=x1b[p2], in_=xs1, func=AF.Copy),
                      r=['xs1'], w=[('x1b', p2)])
                for gg in range(4):
                    pbk = 6 + gg % 2
                    for kk in range(4):
                        k = gg * 4 + kk
                        S.add('pe', lambda e, pbk=pbk, kk=kk, k=k: e.transpose(
                            out=bank(pbk)[:, kk * 128:(kk + 1) * 128], in_=xs1[:, k * 128:(k + 1) * 128],
                            identity=ident_f), r=['xs1', 'ident_f'], w=[('ps', pbk)])
                    evac(x1T[:, gg * 4:(gg + 1) * 4, :], bank(pbk).rearrange("p (a c) -> p a c", a=4),
                         r=[('ps', pbk)], w=['x1T'])
                for gg in range(4):
                    pbk = 6 + gg % 2
                    for jj in range(4):
                        j = gg * 4 + jj
                        for k in range(16):
                            mm(bank(pbk)[:, jj * 128:(jj + 1) * 128], wq_b[:, k, j * 128:(j + 1) * 128],
                               x1T[:, k, :], k == 0, k == 15, r=['x1T', 'wo_b'] + xT_all, w=[('ps', pbk)])
                    evac(qT[:, gg * 4:(gg + 1) * 4, :], bank(pbk).rearrange("p (a c) -> p a c", a=4),
                         r=[('ps', pbk)], w=['qT'])
                for gg in range(4):
                    pbk = gg % 2
                    for jj in range(4):
                        j = gg * 4 + jj
                        mm(bank(pbk)[:, jj * 128:(jj + 1) * 128], qT[:, j, :], kT[j % 2], True, True,
                           r=['qT', ('kT', j % 2)], w=[('ps', pbk)])
                    evac(s_sb[:, gg * 4:(gg + 1) * 4, :], bank(pbk).rearrange("p (a c) -> p a c", a=4),
                         r=[('ps', pbk)], w=['s_sb'])
                for j in range(16):
                    S.add('dve', lambda e, j=j: e.max(out=m16[:, j, 0:8], in_=s_sb[:, j, :]), r=['s_sb'], w=['m16'])
                    S.add('dve', lambda e, j=j: e.max_index(out=i16[:, j, 0:8], in_max=m16[:, j, 0:8],
                                                            in_values=s_sb[:, j, :]), r=['s_sb', 'm16'], w=['i16'])
                    S.add('dve', lambda e, j=j: e.match_replace(out=s_sb[:, j, :], in_to_replace=m16[:, j, 0:8],
                                                                in_values=s_sb[:, j, :], imm_value=NEG_SEL),
                          r=['s_sb', 'm16'], w=['s_sb'])
                    S.add('dve', lambda e, j=j: e.max(out=m16[:, j, 8:16], in_=s_sb[:, j, :]), r=['s_sb'], w=['m16'])
                    S.add('dve', lambda e, j=j: e.max_index(out=i16[:, j, 8:16], in_max=m16[:, j, 8:16],
                                                            in_values=s_sb[:, j, :]), r=['s_sb', 'm16'], w=['i16'])
                m16v = m16.rearrange("p (h c) k -> p h c k", c=2)
                S.add('dve', lambda e: e.tensor_tensor(
                    out=cand.rearrange("p h (a b) -> p h a b", a=16),
                    in0=m16v[:, :, 0, :].unsqueeze(3).to_broadcast([128, 8, 16, 16]),
                    in1=m16v[:, :, 1, :].unsqueeze(2).to_broadcast([128, 8, 16, 16]), op=ALU.add),
                    r=['m16'], w=['x1T', 'qT'])
                S.add('dve', lambda e: e.tensor_copy(out=i16f, in_=i16), r=['i16'], w=['i16f'])
                i16v = i16f.rearrange("p (h c) k -> p h c k", c=2)
                S.add('dve', lambda e: e.tensor_scalar(out=i16v[:, :, 0, :], in0=i16v[:, :, 0, :], scalar1=128.0,
                                                       scalar2=None, op0=ALU.mult), r=['i16f'], w=['i16f'])
                for h in range(8):
                    S.add('dve', lambda e, h=h: e.max(out=ts[:, h, 0:8], in_=cand[:, h, :]), r=['x1T', 'qT'], w=['ts'])
                    S.add('dve', lambda e, h=h: e.max_index(out=pos[:, h, 0:8], in_max=ts[:, h, 0:8],
                                                            in_values=cand[:, h, :]), r=['x1T', 'qT', 'ts'], w=['pos'])
                    S.add('dve', lambda e, h=h: e.match_replace(out=cand[:, h, :], in_to_replace=ts[:, h, 0:8],
                                                                in_values=cand[:, h, :], imm_value=NEG_SEL),
                          r=['x1T', 'qT', 'ts'], w=['x1T', 'qT'])
                    S.add('dve', lambda e, h=h: e.max(out=ts[:, h, 8:16], in_=cand[:, h, :]), r=['x1T', 'qT'], w=['ts'])
                    S.add('dve', lambda e, h=h: e.max_index(out=pos[:, h, 8:16], in_max=ts[:, h, 8:16],
                                                            in_values=cand[:, h, :]), r=['x1T', 'qT', 'ts'], w=['pos'])
                posf = pos.rearrange("p h k -> p (h k)")
                S.add('dve', lambda e: e.tensor_single_scalar(out=pa, in_=posf, scalar=4, op=ALU.logical_shift_right),
                      r=['pos'], w=['pa'])
                S.add('dve', lambda e: e.tensor_single_scalar(out=pb_, in_=posf, scalar=15, op=ALU.bitwise_and),
                      r=['pos'], w=['pb'])
                S.add('dve', lambda e: e.tensor_copy(out=paf, in_=pa), r=['pa'], w=['paf'])
                S.add('dve', lambda e: e.tensor_copy(out=pbf, in_=pb_), r=['pb'], w=['pbf'])
                oh = s_sb.rearrange("p a b -> p (a b)").rearrange("p (m k) -> p m k", k=16)
                oh4 = s_sb.rearrange("p a b -> p (a b)").rearrange("p (h k a) -> p h k a", h=8, k=16)
                for (src, half, red) in ((paf, 0, red1), (pbf, 1, red2)):
                    S.add('dve', lambda e, src=src: e.tensor_tensor(
                        out=oh, in0=src.unsqueeze(2).to_broadcast([128, 128, 16]),
                        in1=iota16.unsqueeze(1).to_broadcast([128, 128, 16]), op=ALU.is_equal),
                        r=['paf', 'pbf', 'iota16', 's_sb'], w=['s_sb'])
                    S.add('dve', lambda e, half=half: e.tensor_tensor(
                        out=oh4, in0=oh4, in1=i16v[:, :, half, :].unsqueeze(2).to_broadcast([128, 8, 16, 16]),
                        op=ALU.mult), r=['s_sb', 'i16f'], w=['s_sb'])
                    S.add('dve', lambda e, red=red: e.tensor_reduce(out=red, in_=oh, axis=AX.X, op=ALU.add),
                          r=['s_sb'], w=[('red', half)])
                S.add('dve', lambda e: e.tensor_tensor(out=red1, in0=red1, in1=red2, op=ALU.add),
                      r=[('red', 0), ('red', 1)], w=[('red', 0)])
                S.add('dve', lambda e: e.tensor_copy(out=eidx[p2], in_=red1), r=[('red', 0)], w=[('eidx', p2)])
                S.add('dve', lambda e: e.tensor_scalar(out=negm, in0=ts[:, :, 0], scalar1=-1.0, scalar2=None, op0=ALU.mult),
                      r=['ts'], w=['negm'])
                for h in range(8):
                    S.add('act', lambda e, h=h: e.activation(out=gts[p2][:, h, :], in_=ts[:, h, :], func=AF.Exp,
                                                             bias=negm[:, h:h + 1], accum_out=zs[:, h:h + 1]),
                          r=['ts', 'negm'], w=[('gts', p2), 'zs'])
                S.add('dve', lambda e: e.reciprocal(out=zs, in_=zs), r=['zs'], w=['zs'])
                S.add('dve', lambda e: e.tensor_tensor(out=gts[p2], in0=gts[p2],
                                                       in1=zs.unsqueeze(2).to_broadcast([128, 8, 16]), op=ALU.mult),
                      r=[('gts', p2), 'zs'], w=[('gts', p2)])

            ring_n = {'i': 0}
            LA = 5

            def tile_items(g):
                p2 = g % 2
                GS = 2

                def mk_u(slot):
                    def gat():
                        rs = ring_n['i'] % 8
                        ring_n['i'] += 1
                        rkeys = [('catT', 2 * rs), ('catT', 2 * rs + 1)]
                        S.add('pool', lambda e: e.indirect_dma_start(
                            out=ring[:, rs, :], out_offset=None, in_=uvbf[:, :],
                            in_offset=bass.IndirectOffsetOnAxis(ap=eidx[p2][:, slot:slot + 1], axis=0)),
                            r=[('eidx', p2)] + tbl_all, w=rkeys, dma=True)
                        return rs

                    def con(rs):
                        ring_of[slot] = rs
                        rkeys = [('catT', 2 * rs), ('catT', 2 * rs + 1)]
                        S.add('dve', lambda e: e.tensor_tensor(
                            out=ring[:, rs, 0:D], in0=ring[:, rs, 0:D], in1=x1b[p2], op=ALU.mult),
                            r=rkeys + [('x1b', p2)], w=[rkeys[0]])
                        S.add('act', lambda e: e.activation(
                            out=ring[:, rs, 0:D], in_=ring[:, rs, 0:D], func=AF.Copy,
                            accum_out=hv[:, slot:slot + 1]),
                            r=[rkeys[0]], w=[rkeys[0], ('hv', slot // GS)])
                    return (gat, con)

                def mk_av(grp):
                    def con(_):
                        sl_ = slice(grp * GS, (grp + 1) * GS)
                        S.add('act', lambda e: e.activation(out=av[:, sl_], in_=hv[:, sl_], func=AF.Gelu),
                              r=[('hv', grp)], w=[('av', grp)])
                        S.add('dve', lambda e: e.tensor_tensor(
                            out=av[:, sl_], in0=av[:, sl_], in1=gts[p2].rearrange("p h k -> p (h k)")[:, sl_],
                            op=ALU.mult), r=[('av', grp), ('gts', p2)], w=[('av', grp)])
                        for slot in range(grp * GS, (grp + 1) * GS):
                            rs = ring_of[slot]
                            rkeys = [('catT', 2 * rs), ('catT', 2 * rs + 1)]
                            d = dg[slot % 4]
                            S.add('act', lambda e, d=d, slot=slot: e.activation(
                                out=d, in_=ident_b, func=AF.Copy, scale=av[:, slot:slot + 1]),
                                r=['ident_b', ('av', grp)], w=[('dg', slot % 4)])
                            for cb in range(4):
                                mm(bank(2 + cb), d, ring[:, rs, D + cb * 512:D + (cb + 1) * 512], slot == 0, slot == 127,
                                   r=[('dg', slot % 4)] + rkeys, w=[('ps', 2 + cb)])
                    return (None, con)

                ring_of = {}
                ngrp = 128 // GS
                items = [mk_u(sl_) for sl_ in range(GS)]
                for grp in range(ngrp):
                    if grp + 1 < ngrp:
                        items.extend(mk_u(sl_) for sl_ in range((grp + 1) * GS, (grp + 2) * GS))
                    items.append(mk_av(grp))
                items.append((None, lambda _: fin(g)))
                return items

            def fin(g):
                S.add('sp', lambda e: e.dma_start(out=rb, in_=x1s[g * 128:(g + 1) * 128, :]), w=['rb'], dma=True)
                for cb in range(4):
                    S.add('dve', lambda e, cb=cb: e.scalar_tensor_tensor(
                        out=rb[:, cb * 512:(cb + 1) * 512], in0=rb[:, cb * 512:(cb + 1) * 512], scalar=ALPHA,
                        in1=bank(2 + cb), op0=ALU.mult, op1=ALU.add), r=['rb', ('ps', 2 + cb)], w=['rb'])
                    S.add('dve', lambda e, cb=cb: e.bn_stats(out=st6[:, cb, :], in_=rb[:, cb * 512:(cb + 1) * 512]),
                          r=['rb'], w=['st6'])
                S.add('dve', lambda e: e.bn_aggr(out=mv, in_=st6), r=['st6'], w=['mv'])
                S.add('act', lambda e: e.activation(out=rstd, in_=mv[:, 1:2], func=AF.Sqrt, bias=eps_ln),
                      r=['mv', 'eps'], w=['rstd'])
                S.add('dve', lambda e: e.reciprocal(out=rstd, in_=rstd), r=['rstd'], w=['rstd'])
                S.add('dve', lambda e: e.tensor_scalar(out=rb, in0=rb, scalar1=mv[:, 0:1], scalar2=rstd,
                                                       op0=ALU.subtract, op1=ALU.mult), r=['rb', 'mv', 'rstd'], w=['rb'])
                S.add('dve', lambda e: e.tensor_tensor(out=rb, in0=rb, in1=lng, op=ALU.mult), r=['rb', 'lng'], w=['rb'])
                S.add('dve', lambda e: e.tensor_tensor(out=rb, in0=rb, in1=lnb, op=ALU.add), r=['rb', 'lnb'], w=['rb'])
                S.add('sp', lambda e: e.dma_start(out=out[g * 128:(g + 1) * 128, :], in_=rb), r=['rb'], w=[('out', g)], dma=True)

            def record_stage1(g):
                rec = []
                orig = S.add
                S.add = lambda *a_, **k_: rec.append((a_, k_))
                try:
                    stage1(g)
                finally:
                    S.add = orig
                return rec

            ngl = NG if stage == "full" else int(stage[2:])
            stage1(0)
            for g in range(ngl):
                items = tile_items(g)
                rec = record_stage1(g + 1) if g + 1 < ngl else []
                gl = [i for i, it in enumerate(items) if it[0] is not None]
                per = (len(rec) + 199) // 200 if rec else 0
                slots = {}
                gi = 0
                ri = 0
                ngat = 0
                for ci, (gf, cf) in enumerate(items):
                    while gi < len(gl) and ngat < 8:
                        slots[gl[gi]] = items[gl[gi]][0]()
                        gi += 1
                        ngat += 1
                    if gf is not None:
                        cf(slots.pop(ci))
                    else:
                        cf(None)
                        if ci < len(items) - 1:
                            ngat -= 2
                    if ci >= 8:
                        for _ in range(per):
                            if ri < len(rec):
                                a_, k_ = rec[ri]
                                S.add(*a_, **k_)
                                ri += 1
                while ri < len(rec):
                    a_, k_ = rec[ri]
                    S.add(*a_, **k_)
                    ri += 1

        try:
            for b in range(NSEQ):
                if not stage.startswith("Bo"):
                    phase_a(b)
            if stage == "full" or stage.startswith("B"):
                phase_b()
        except _Stop:
            pass

        S.finalize(block, st)
        print("ops per engine:", S.stats, flush=True)
    return nc


_CACHE = {}


def kernel(**inputs):
    stage = inputs.pop("_stage", "full")
    n = 8
    x = np.ascontiguousarray(inputs["x"], dtype=np.float32)
    shared = {}
    for k in ("w_in", "w_o", "lambda_q1", "lambda_k1", "lambda_q2", "lambda_k2", "subln_g", "ln1_g", "ln1_b",
              "peer_wq", "peer_k1", "peer_k2", "peer_u", "peer_v", "ln2_g", "ln2_b"):
        shared[k] = np.ascontiguousarray(np.asarray(inputs[k], dtype=np.float32)[0])
    for k in ("lambda_q1", "lambda_k1", "lambda_q2", "lambda_k2", "subln_g", "ln1_g", "ln1_b", "ln2_g", "ln2_b"):
        shared[k] = shared[k].reshape(1, -1)
    if stage not in _CACHE:
        _CACHE[stage] = build_program(stage)
    nc = _CACHE[stage]
    in_maps = []
    for c in range(n):
        m = dict(shared)
        m["x"] = x[c * NSEQ:(c + 1) * NSEQ]
        in_maps.append(m)
    res = run_bass_kernel_spmd(nc, in_maps, core_ids=list(range(n)))
    outs = [np.asarray(r["out"]).reshape(NSEQ, S_LEN, D) for r in res.results]
    return np.concatenate(outs, axis=0).astype(np.float32)
```

```python
import math
from contextlib import ExitStack

import numpy as np
import concourse.bass as bass
import concourse.mybir as mybir
from concourse.bass_utils import run_bass_kernel_spmd

F32 = mybir.dt.float32
BF16 = mybir.dt.bfloat16
I32 = mybir.dt.int32
U32 = mybir.dt.uint32
AF = mybir.ActivationFunctionType
ALU = mybir.AluOpType
AX = mybir.AxisListType

D = 2048
S_LEN = 2048
NSEQ = 2
NTT = S_LEN // 128
IN_COLS = 5456
C_DQ, C_DK, C_DV, C_SQ, C_SK, C_SV, C_IQ, C_IK, C_IW = 0, 1024, 2048, 3072, 4096, 4224, 4352, 5376, 5440
ALPHA = 2.0 ** 0.25
LAM_INIT = 0.8 - 0.6 * math.exp(0.0)
LN_EPS = 1e-5
RMS_EPS = 1e-5
_sl = 2.0 ** (-8.0 * np.arange(1, 17) / 16)
SLOPES_DIFF = [float(v) for v in _sl[0::2]]
SLOPES_DSA = [float(v) for v in _sl[1::2]]
SKIP_EXP = 80.0
NEG_MASK = -1.0e30
NEG_SEL = -3.0e38
QB_DSA = 256


class Sched:
    ENGS = ('pe', 'act', 'dve', 'pool', 'sp')
    EPOCH = 20000

    def __init__(self, nc, n_dma_sems=24):
        self.nc = nc
        self.ops = []
        self.res_w = {}
        self.res_r = {}
        self.n_dma_sems = n_dma_sems

    def add(self, eng, fn, r=(), w=(), dma=False):
        idx = len(self.ops)
        deps = set()
        for x in r:
            lw = self.res_w.get(x)
            if lw is not None:
                deps.add(lw)
        for x in w:
            lw = self.res_w.get(x)
            if lw is not None:
                deps.add(lw)
            deps.update(self.res_r.get(x, ()))
        for x in r:
            lst = self.res_r.setdefault(x, [])
            if not dma:
                lst[:] = [o for o in lst if self.ops[o]['dma'] or self.ops[o]['eng'] != eng]
            lst.append(idx)
        for x in w:
            self.res_w[x] = idx
            self.res_r[x] = []
        deps.discard(idx)
        self.ops.append(dict(eng=eng, fn=fn, deps=deps, dma=dma, idx=idx))
        return idx

    def finalize(self, block, stack):
        nc = self.nc
        ops = self.ops
        for o in ops:
            o['sig'] = False
        for o in ops:
            for d in o['deps']:
                od = ops[d]
                if od['dma']:
                    continue
                if od['eng'] == o['eng'] and o['eng'] == 'pe' and not o['dma']:
                    continue
                od['sig'] = True
        cnt = {e: 0 for e in self.ENGS}
        nsem = {e: 1 for e in self.ENGS}
        for o in ops:
            if o['dma'] or not o['sig']:
                continue
            e = o['eng']
            c = cnt[e]
            o['ep'] = c // self.EPOCH
            o['val'] = c % self.EPOCH + 1
            cnt[e] = c + 1
            nsem[e] = o['ep'] + 1
        sems = {e: [stack.enter_context(nc.semaphore(f"s_{e}_{i}")) for i in range(nsem[e])]
                for e in self.ENGS}
        dsems = {}
        dstate = {}
        for o in ops:
            if not o['dma']:
                continue
            e = o['eng']
            if e not in dsems:
                dsems[e] = [stack.enter_context(nc.semaphore(f"d_{e}_{i}"))
                            for i in range(self.n_dma_sems)]
                dstate[e] = dict(n=0, vals=[0] * self.n_dma_sems, last=[None] * self.n_dma_sems)
            st = dstate[e]
            k = st['n'] % self.n_dma_sems
            st['n'] += 1
            o['dsem'] = dsems[e][k]
            o['prev_on_sem'] = st['last'][k]
            st['vals'][k] += 16
            o['dval'] = st['vals'][k]
            st['last'][k] = o['idx']
        per_eng = {e: [o for o in ops if o['eng'] == e] for e in self.ENGS}
        self.stats = {e: len(per_eng[e]) for e in self.ENGS}

        def make(e):
            def body(engine):
                waited = {}
                waited_dma = set()
                for o in per_eng[e]:
                    deps = set(o['deps'])
                    if o['dma'] and o['prev_on_sem'] is not None:
                        deps.add(o['prev_on_sem'])
                    need = {}
                    for d in sorted(deps):
                        od = ops[d]
                        if od['dma']:
                            if d in waited_dma:
                                continue
                            waited_dma.add(d)
                            engine.wait_ge(od['dsem'], od['dval'])
                            continue
                        if od['eng'] == e and e == 'pe' and not o['dma']:
                            continue
                        key = (od['eng'], od['ep'])
                        if waited.get(key, 0) >= od['val']:
                            continue
                        need[key] = max(need.get(key, 0), od['val'])
                    for key, v in need.items():
                        engine.wait_ge(sems[key[0]][key[1]], v)
                        waited[key] = v
                    ins = o['fn'](engine)
                    if o['dma']:
                        ins.then_inc(o['dsem'], 16)
                    elif o['sig']:
                        ins.then_inc(sems[e][o['ep']], 1)
                if e in dstate:
                    st = dstate[e]
                    for k in range(self.n_dma_sems):
                        if st['vals'][k] > 0:
                            engine.wait_ge(dsems[e][k], st['vals'][k])
            return body

        block.tensor(make('pe'))
        block.scalar(make('act'))
        block.vector(make('dve'))
        block.gpsimd(make('pool'))
        block.sync(make('sp'))


class _Stop(Exception):
    pass


class Arena:
    def __init__(self, nc, st, name, kb):
        self.t = st.enter_context(nc.sbuf_tensor(name, [128, kb * 256], F32))
        self.words = kb * 256
        self.off = 0

    def reset(self, off=0):
        self.off = off

    def alloc(self, dtype, *free):
        n = 1
        for f in free:
            n *= f
        esz = 4 if dtype in (F32, I32, U32) else 2
        words = (n * esz + 3) // 4
        assert self.off + words <= self.words, (self.off, words, self.words)
        ap = self.t[:, self.off:self.off + words]
        self.off += words
        if dtype != F32:
            ap = ap.bitcast(dtype)
        if len(free) == 2:
            ap = ap.rearrange("p (a b) -> p a b", a=free[0])
        elif len(free) == 3:
            ap = ap.rearrange("p (a b c) -> p a b c", a=free[0], b=free[1])
        return ap


def build_program(stage="full"):
    nc = bass.Bass("TRN2", target_bir_lowering=False)
    x = nc.dram_tensor("x", [NSEQ, S_LEN, D], F32, kind="ExternalInput").ap()
    w_in = nc.dram_tensor("w_in", [D, IN_COLS], F32, kind="ExternalInput").ap()
    w_o = nc.dram_tensor("w_o", [D, D], F32, kind="ExternalInput").ap()
    lq1 = nc.dram_tensor("lambda_q1", [1, 64], F32, kind="ExternalInput").ap()
    lk1 = nc.dram_tensor("lambda_k1", [1, 64], F32, kind="ExternalInput").ap()
    lq2 = nc.dram_tensor("lambda_q2", [1, 64], F32, kind="ExternalInput").ap()
    lk2 = nc.dram_tensor("lambda_k2", [1, 64], F32, kind="ExternalInput").ap()
    subln_g = nc.dram_tensor("subln_g", [1, 128], F32, kind="ExternalInput").ap()
    ln1_g = nc.dram_tensor("ln1_g", [1, D], F32, kind="ExternalInput").ap()
    ln1_b = nc.dram_tensor("ln1_b", [1, D], F32, kind="ExternalInput").ap()
    peer_wq = nc.dram_tensor("peer_wq", [D, D], F32, kind="ExternalInput").ap()
    peer_k1 = nc.dram_tensor("peer_k1", [128, 128], F32, kind="ExternalInput").ap()
    peer_k2 = nc.dram_tensor("peer_k2", [128, 128], F32, kind="ExternalInput").ap()
    peer_u = nc.dram_tensor("peer_u", [16384, D], F32, kind="ExternalInput").ap()
    peer_v = nc.dram_tensor("peer_v", [16384, D], F32, kind="ExternalInput").ap()
    ln2_g = nc.dram_tensor("ln2_g", [1, D], F32, kind="ExternalInput").ap()
    ln2_b = nc.dram_tensor("ln2_b", [1, D], F32, kind="ExternalInput").ap()
    out = nc.dram_tensor("out", [NSEQ * S_LEN, D], F32, kind="ExternalOutput").ap()
    if stage.startswith("Bo"):
        x1s = nc.dram_tensor("x1s", [NSEQ * S_LEN, D], F32, kind="ExternalInput").ap()
    else:
        x1s = nc.dram_tensor("x1s", [NSEQ * S_LEN, D], F32).ap()
    dbg = None
    if stage.startswith("dbg"):
        dbg = nc.dram_tensor("dbg", [128, 16 * 2048], BF16, kind="ExternalOutput").ap()
    uvbf = nc.dram_tensor("uvbf", [16384, 2 * D], BF16).ap()
    sq_d = nc.dram_tensor("sq_d", [8, 128, S_LEN], BF16).ap()
    iq_d = nc.dram_tensor("iq_d", [8, 128, S_LEN], BF16).ap()

    w_in_v = w_in.rearrange("(k p) c -> p k c", p=128)

    with ExitStack() as st:
        ar = Arena(nc, st, "arena", 207)
        ps = st.enter_context(nc.psum_tensor("ps", [128, 8, 512], F32))
        block = st.enter_context(nc.Block())
        S = Sched(nc)

        def bank(i):
            return ps[:, i, :]

        big1 = ar.alloc(BF16, 16, 2048)
        big2 = ar.alloc(BF16, 16, 2048)
        ident_f = ar.alloc(F32, 128)
        ident_b = ar.alloc(BF16, 128)
        ones_b = ar.alloc(BF16, 128)
        onesf = ar.alloc(BF16, 128)
        small = ar.alloc(F32, 16)
        workB0 = ar.off
        val = ar.alloc(F32, 512)
        valc = ar.alloc(F32, 512)
        lamv = ar.alloc(F32, 4, 64)
        lamt = ar.alloc(F32, 2, 64)
        neg_lam = small[:, 0:1]
        g08 = small[:, 1:2]
        eps_ln = small[:, 2:3]
        esum = small[:, 4:6]
        work0 = ar.off

        xT = big1
        catT = big2

        S.add('pool', lambda e: e.memset(ident_f, 0.0), w=['ident_f'])
        S.add('pool', lambda e: e.affine_select(out=ident_f, in_=ident_f, pattern=[[-1, 128]],
                                                 compare_op=ALU.not_equal, fill=1.0, base=0,
                                                 channel_multiplier=1), r=['ident_f'], w=['ident_f'])
        S.add('pool', lambda e: e.tensor_copy(out=ident_b, in_=ident_f), r=['ident_f'], w=['ident_b'])
        S.add('pool', lambda e: e.memset(ones_b, 1.0), w=['ones_b'])
        S.add('pool', lambda e: e.memset(onesf, 1.0 / 128.0), w=['onesf'])
        S.add('pool', lambda e: e.memset(eps_ln, LN_EPS), w=['eps'])
        S.add('pool', lambda e: e.iota(val, [[1, 512]], base=0, channel_multiplier=-1,
                                       allow_small_or_imprecise_dtypes=True), w=['val'])
        S.add('pool', lambda e: e.tensor_scalar(out=valc, in0=val, scalar1=0.0, scalar2=None, op0=ALU.max),
              r=['val'], w=['valc'])
        for i, t in enumerate((lq1, lk1, lq2, lk2)):
            S.add('sp', lambda e, i=i, t=t: e.dma_start(out=lamv[:, i, :], in_=t[0:1, :].to_broadcast([128, 64])),
                  w=[('lamv', i)], dma=True)
        S.add('sp', lambda e: e.dma_start(out=g08, in_=subln_g.rearrange("o (p u) -> (o p) u", u=1)),
              w=['g08'], dma=True)
        for i in range(2):
            S.add('dve', lambda e, i=i: e.tensor_tensor(out=lamt[:, i, :], in0=lamv[:, 2 * i, :],
                                                        in1=lamv[:, 2 * i + 1, :], op=ALU.mult),
                  r=[('lamv', 2 * i), ('lamv', 2 * i + 1)], w=[('lamt', i)])
            S.add('dve', lambda e, i=i: e.reduce_sum(out=esum[:, i:i + 1], in_=lamt[:, i, :], axis=AX.X),
                  r=[('lamt', i)], w=[('esum', i)])
        S.add('act', lambda e: e.activation(out=esum, in_=esum, func=AF.Exp),
              r=[('esum', 0), ('esum', 1)], w=['esum_e'])
        S.add('dve', lambda e: e.tensor_tensor(out=neg_lam, in0=esum[:, 1:2], in1=esum[:, 0:1], op=ALU.subtract),
              r=['esum_e'], w=['neg_lam'])
        S.add('dve', lambda e: e.tensor_scalar(out=neg_lam, in0=neg_lam, scalar1=-LAM_INIT, scalar2=None, op0=ALU.add),
              r=['neg_lam'], w=['neg_lam'])
        S.add('dve', lambda e: e.tensor_scalar(out=g08, in0=g08, scalar1=1.0 - LAM_INIT, scalar2=None, op0=ALU.mult),
              r=['g08'], w=['g08'])

        rr = {'cp': 0}

        def evac(out_ap, in_ap, r, w):
            rr['cp'] += 1
            if rr['cp'] % 2:
                S.add('act', lambda e: e.activation(out=out_ap, in_=in_ap, func=AF.Copy), r=r, w=w)
            else:
                S.add('dve', lambda e: e.tensor_copy(out=out_ap, in_=in_ap), r=r, w=w)

        def mm(out_ap, lhsT, rhs, start, stop, r, w):
            S.add('pe', lambda e: e.matmul(out_ap, lhsT=lhsT, rhs=rhs, start=start, stop=stop), r=r, w=w)

        wch_n = {'i': 0}
        TCH = 1024
        tbl_chunks = [(src, c0, r0) for (src, c0) in ((peer_u, 0), (peer_v, D))
                      for r0 in range(0, 16384, TCH)]
        tbl_state = {'i': 0}

        def emit_tbl_chunk():
            if not (stage == "full" or stage.startswith("B")) or tbl_state['i'] >= len(tbl_chunks):
                tbl_state['i'] = max(tbl_state['i'], len(tbl_chunks)) if not (stage == "full" or stage.startswith("B")) else tbl_state['i']
                return
            src, c0, r0 = tbl_chunks[tbl_state['i']]
            key = ('tbl', tbl_state['i'])
            tbl_state['i'] += 1
            S.add('pool', lambda e: e.dma_start(out=uvbf[r0:r0 + TCH, c0:c0 + D], in_=src[r0:r0 + TCH, :]),
                  w=[key], dma=True)

        def phase_a(b):
            gate = [('x1s', b - 1, tt) for tt in range(NTT)] if b > 0 else []
            ar.reset(work0)
            xs = [ar.alloc(F32, 2048) for _ in range(2)]
            a1_end = ar.off
            for tt in range(NTT):
                xb = xs[tt % 2]
                S.add('sp', lambda e, xb=xb, tt=tt: e.dma_start(out=xb, in_=x[b, tt * 128:(tt + 1) * 128, :]),
                      w=[('xs', tt % 2)], dma=True)
                for g in range(4):
                    pb = 6 + g % 2
                    for kk in range(4):
                        k = g * 4 + kk
                        S.add('pe', lambda e, pb=pb, kk=kk, k=k, xb=xb: e.transpose(
                            out=bank(pb)[:, kk * 128:(kk + 1) * 128], in_=xb[:, k * 128:(k + 1) * 128],
                            identity=ident_f), r=[('xs', tt % 2), 'ident_f'], w=[('ps', pb)])
                    evac(xT[:, g * 4:(g + 1) * 4, tt * 128:(tt + 1) * 128],
                         bank(pb).rearrange("p (a c) -> p a c", a=4), r=[('ps', pb)], w=[('xT', tt)])
            xT_all = [('xT', tt) for tt in range(NTT)]
            gate = gate + xT_all
            if stage == "dbgA1":
                S.add('sp', lambda e: e.dma_start(out=dbg, in_=big1.rearrange("p a b -> p (a b)")), r=xT_all, dma=True)
                raise _Stop()

            ar.reset(work0)
            wch = [ar.alloc(BF16, 16, 128) for _ in range(3)]
            tdiag = [ar.alloc(BF16, 512) for _ in range(2)]
            toff = [ar.alloc(BF16, 512) for _ in range(2)]
            Eb = [ar.alloc(BF16, 512) for _ in range(2)]
            Pb = [ar.alloc(BF16, 512) for _ in range(2)]
            lnz = [ar.alloc(F32, 512)]
            att0 = ar.off
            lnz.append(ar.alloc(F32, 512))
            fA = ar.alloc(F32, 512)
            fB = ar.alloc(F32, 512)
            fD = ar.alloc(F32, 512)
            fS = ar.alloc(BF16, 512)
            fR = ar.alloc(F32, 512)

            def load_w(c0, ncols=128, dst_col=0, slot=None, new=True, slots=(0, 1, 2)):
                if new:
                    wch_n['i'] += 1
                sl = slots[wch_n['i'] % len(slots)]
                dst = wch[sl]
                S.add('pool', lambda e: e.dma_start(out=dst[:, :, dst_col:dst_col + ncols],
                                                    in_=w_in_v[:, :, c0:c0 + ncols]),
                      r=gate, w=[('wch', sl)], dma=True)
                if b == NSEQ - 1 and new:
                    emit_tbl_chunk()
                return sl

            def proj_fm(sl, dst, ncols_t, t0, r_extra=(), wkey=None, m=128, banks=(6, 7)):
                nb = (ncols_t + 511) // 512
                for tb in range(nb):
                    n = min(512, ncols_t - tb * 512)
                    pb = banks[rr['cp'] % 2]
                    for k in range(16):
                        mm(bank(pb)[:m, :n], wch[sl][:, k, :m], xT[:, k, t0 + tb * 512:t0 + tb * 512 + n],
                           k == 0, k == 15, r=[('wch', sl)] + xT_all, w=[('ps', pb)])
                    evac(dst[:m, tb * 512:tb * 512 + n], bank(pb)[:m, :n], r=[('ps', pb)], w=[wkey])

            def proj_tm(sl, dst, ncols, wkey):
                per = 512 // ncols
                per = min(per, 16)
                for g in range(16 // per):
                    pb = 6 + rr['cp'] % 2
                    for j in range(per):
                        tt = g * per + j
                        for k in range(16):
                            mm(bank(pb)[:, j * ncols:(j + 1) * ncols], xT[:, k, tt * 128:(tt + 1) * 128],
                               wch[sl][:, k, :ncols], k == 0, k == 15, r=[('wch', sl)] + xT_all, w=[('ps', pb)])
                    evac(dst[:, g * per:(g + 1) * per, :],
                         bank(pb)[:, :per * ncols].rearrange("p (a c) -> p a c", a=per),
                         r=[('ps', pb)], w=[wkey])

            def make_T(slope, par):
                S.add('act', lambda e: e.activation(out=tdiag[par], in_=valc, func=AF.Exp, scale=-slope),
                      r=['valc'] + gate, w=[('tdiag', par)])
                S.add('pool', lambda e: e.affine_select(out=tdiag[par], in_=tdiag[par], pattern=[[1, 512]],
                                                        compare_op=ALU.is_ge, fill=0.0, base=0,
                                                        channel_multiplier=-1),
                      r=[('tdiag', par)], w=[('tdiag', par)])
                S.add('act', lambda e: e.activation(out=toff[par], in_=val, func=AF.Exp, scale=-slope,
                                                    bias=-slope * 128.0),
                      r=['val'] + gate, w=[('toff', par)])

            def attn_block(slope, par, q0, QB, kq_list, v_of, sel_of, scale, o_banks, z_banks, n_c, hook=None):
                first_tile = q0 // 128
                tiles = []
                for stile in range(0, first_tile + QB // 128):
                    if stile < first_tile:
                        r_ = first_tile - stile
                        if slope * 128.0 * (r_ - 1) > SKIP_EXP:
                            continue
                    tiles.append(stile)
                items = [(c, stile) for c in range(n_c) for stile in tiles]
                pend = None
                for i, (c, stile) in enumerate(items):
                    sb = i % 2
                    j = stile - first_tile
                    col0 = 128 * j if j > 0 else 0
                    n = QB - col0
                    kT_ap, qT_ap, rk = kq_list[c](stile, col0, n)
                    mm(bank(sb)[:, :n], kT_ap, qT_ap, True, True, r=rk, w=[('ps', sb)])
                    if j >= 0:
                        bias = 0.0
                        tm = tdiag[par]
                        tkey = ('tdiag', par)
                    else:
                        bias = -slope * 128.0 * (-j - 1)
                        tm = toff[par]
                        tkey = ('toff', par)
                    S.add('act', lambda e, sb=sb, n=n, bias=bias: e.activation(
                        out=Eb[sb][:, :n], in_=bank(sb)[:, :n], func=AF.Exp, scale=scale, bias=bias),
                        r=[('ps', sb)], w=[('E', sb)])
                    S.add('dve', lambda e, sb=sb, n=n, tm=tm: e.tensor_tensor(
                        out=Pb[sb][:, :n], in0=Eb[sb][:, :n], in1=tm[:, :n], op=ALU.mult),
                        r=[('E', sb), tkey], w=[('P', sb)])
                    if sel_of is not None:
                        sel_ap, sel_key = sel_of(stile, col0, n)
                        S.add('pool', lambda e, sb=sb, n=n, sel_ap=sel_ap: e.tensor_tensor(
                            out=Pb[sb][:, :n], in0=Pb[sb][:, :n], in1=sel_ap, op=ALU.mult),
                            r=[('P', sb), sel_key], w=[('P', sb)])
                    if pend is not None:
                        pend()
                    first = (stile == tiles[0])
                    last = (stile == tiles[-1])
                    v_ap, v_key = v_of(stile)

                    def pv(c=c, sb=sb, col0=col0, n=n, first=first, last=last, v_ap=v_ap, v_key=v_key):
                        mm(bank(o_banks[c])[:, col0:col0 + n], v_ap, Pb[sb][:, :n], first, last,
                           r=[('P', sb), v_key], w=[('ps', o_banks[c])])
                        mm(bank(z_banks[c])[:, col0:col0 + n], ones_b, Pb[sb][:, :n], first, last,
                           r=[('P', sb), 'ones_b'], w=[('ps', z_banks[c])])
                    pend = pv
                    if hook is not None:
                        hook()
                pend()

            def recip_z(c, zb, QB):
                S.add('act', lambda e: e.activation(out=lnz[c][:, :QB], in_=bank(zb)[:, :QB], func=AF.Ln),
                      r=[('ps', zb)], w=[('lnz', c)])
                S.add('act', lambda e: e.activation(out=lnz[c][:, :QB], in_=lnz[c][:, :QB], func=AF.Exp, scale=-1.0),
                      r=[('lnz', c)], w=[('lnz', c)])

            qT_h = ar.alloc(BF16, 2048)
            kT_h = ar.alloc(BF16, 2048)
            v_h = ar.alloc(BF16, 16, 128)
            scale_d = 64 ** -0.5
            wch.extend(ar.alloc(BF16, 16, 128) for _ in range(3))
            S6 = (0, 1, 2, 3, 4, 5)
            nxt = [load_w(C_DQ, slots=S6), load_w(C_DK, slots=S6), load_w(C_DV, slots=S6)]
            for h in range(8):
                par = h % 2
                slope = SLOPES_DIFF[h]
                make_T(slope, par)
                slq, slk, slv = nxt
                proj_fm(slq, qT_h, 2048, 0, wkey='qT_h')
                proj_fm(slk, kT_h, 2048, 0, wkey='kT_h')
                proj_tm(slv, v_h, 128, wkey='v_h')
                if h + 1 < 8:
                    nxt = [load_w(C_DQ + (h + 1) * 128, slots=S6), load_w(C_DK + (h + 1) * 128, slots=S6),
                           load_w(C_DV + (h + 1) * 128, slots=S6)]
                if stage == "dbgP":
                    S.add('sp', lambda e: e.dma_start(out=dbg[:, 0:2048], in_=qT_h), r=['qT_h'], dma=True)
                    S.add('sp', lambda e: e.dma_start(out=dbg[:, 2048:4096], in_=kT_h), r=['kT_h'], dma=True)
                    S.add('sp', lambda e: e.dma_start(out=dbg[:, 4096:6144], in_=v_h.rearrange("p a b -> p (a b)")), r=['v_h'], dma=True)
                    S.add('sp', lambda e: e.dma_start(out=dbg[:, 6144:6656], in_=tdiag[par]), r=[('tdiag', par)], dma=True)
                    S.add('sp', lambda e: e.dma_start(out=dbg[:, 6656:7168], in_=toff[par]), r=[('toff', par)], dma=True)
                    raise _Stop()
                for qb in range(4):
                    q0 = qb * 512

                    def kq(c):
                        def f(stile, col0, n):
                            return (kT_h[c * 64:(c + 1) * 64, stile * 128:(stile + 1) * 128],
                                    qT_h[c * 64:(c + 1) * 64, q0 + col0:q0 + col0 + n],
                                    ['qT_h', 'kT_h'])
                        return f
                    attn_block(slope, par, q0, 512, [kq(0), kq(1)],
                               lambda stile: (v_h[:, stile, :], 'v_h'), None, scale_d,
                               [2, 3], [4, 5], 2)
                    recip_z(0, 4, 512)
                    recip_z(1, 5, 512)
                    S.add('dve', lambda e: e.tensor_tensor(out=fA, in0=bank(2), in1=lnz[0], op=ALU.mult),
                          r=[('ps', 2), ('lnz', 0)], w=['fA'])
                    S.add('dve', lambda e: e.tensor_tensor(out=fB, in0=bank(3), in1=lnz[1], op=ALU.mult),
                          r=[('ps', 3), ('lnz', 1)], w=['fB'])
                    S.add('dve', lambda e: e.scalar_tensor_tensor(out=fD, in0=fB, scalar=neg_lam, in1=fA,
                                                                  op0=ALU.mult, op1=ALU.add),
                          r=['fA', 'fB', 'neg_lam'], w=['fD'])
                    S.add('pool', lambda e: e.tensor_tensor(out=fS, in0=fD, in1=fD, op=ALU.mult),
                          r=['fD'], w=['fS'])
                    pbq = 6 + rr['cp'] % 2
                    rr['cp'] += 1
                    mm(bank(pbq), onesf, fS, True, True, r=['onesf', 'fS'], w=[('ps', pbq)])
                    S.add('act', lambda e, pbq=pbq: e.activation(out=fR, in_=bank(pbq), func=AF.Ln, bias=eps_ln),
                          r=[('ps', pbq), 'eps'], w=['fR'])
                    S.add('act', lambda e: e.activation(out=fR, in_=fR, func=AF.Exp, scale=-0.5),
                          r=['fR'], w=['fR'])
                    S.add('dve', lambda e, h=h, q0=q0: e.scalar_tensor_tensor(
                        out=catT[:, h, q0:q0 + 512], in0=fD, scalar=g08, in1=fR, op0=ALU.mult, op1=ALU.mult),
                        r=['fD', 'fR', 'g08'], w=[('catT', h)])
                if stage == "dbgA2" and h == 0:
                    S.add('sp', lambda e: e.dma_start(out=dbg[:, 0:2048], in_=big2[:, 0, :]), r=[('catT', 0)], dma=True)
                    raise _Stop()
            if stage == "dbgA2all":
                S.add('sp', lambda e: e.dma_start(out=dbg[:, 0:8 * 2048], in_=big2[:, 0:8, :].rearrange("p a b -> p (a b)")), r=[('catT', hh) for hh in range(8)], dma=True)
                raise _Stop()

            ar.reset(att0)
            skT = ar.alloc(BF16, 2048)
            ikT2 = ar.alloc(BF16, 2048)
            sv = ar.alloc(BF16, 16, 128)
            iwa = ar.alloc(F32, 16, 16)
            iws = ar.alloc(F32, 16, 16)
            selT = ar.alloc(BF16, 16, QB_DSA)
            isc = ar.alloc(F32, 2048)
            selb = [ar.alloc(BF16, 2048) for _ in range(QB_DSA // 128)]
            iqT = ar.alloc(BF16, 8, QB_DSA)
            sq_blk = ar.alloc(BF16, 8, QB_DSA)
            rtmp = [ar.alloc(F32, 512) for _ in range(2)]
            m8 = ar.alloc(F32, 8)
            bs = m8
            NBIS = 24

            LIVE = (0, 1)
            BG = (2,)
            sl = load_w(C_SK)
            proj_fm(sl, skT, 2048, 0, wkey='skT')
            sl = load_w(C_IK, 64, 0)
            load_w(C_IK, 64, 64, new=False)
            proj_fm(sl, ikT2, 2048, 0, wkey='ikT2')
            sl = load_w(C_SV)
            proj_tm(sl, sv, 128, wkey='sv')
            sl = load_w(C_IW, 16, 0)
            iw = isc.rearrange("p (a c) -> p a c", a=16)[:, :, 0:16]
            proj_tm(sl, iw, 16, wkey=('isc', 0))
            S.add('dve', lambda e: e.tensor_scalar(out=iws, in0=iw, scalar1=0.0, scalar2=None, op0=ALU.is_ge),
                  r=[('isc', 0)], w=['iws'])
            S.add('dve', lambda e: e.tensor_scalar(out=iws, in0=iws, scalar1=2.0, scalar2=-1.0, op0=ALU.mult, op1=ALU.add),
                  r=['iws'], w=['iws'])
            S.add('dve', lambda e: e.tensor_tensor(out=iwa, in0=iw, in1=iws, op=ALU.mult),
                  r=[('isc', 0), 'iws'], w=['iwa'])
            scale_s = 128 ** -0.5
            NQT = QB_DSA // 128
            NQB = S_LEN // QB_DSA
            isc_all = [('isc', ch) for ch in range(4)]
            for i_ in range(16):
                tmpT = selb[i_ % 2]
                c0_ = (C_SQ if i_ < 8 else C_IQ) + (i_ % 8) * 128
                dst_ = (sq_d if i_ < 8 else iq_d)[i_ % 8]
                sl = load_w(c0_)
                proj_fm(sl, tmpT, 2048, 0, wkey=('selb', i_ % 2))
                S.add('sp', lambda e, dst_=dst_, tmpT=tmpT: e.dma_start(out=dst_, in_=tmpT),
                      r=[('selb', i_ % 2)], w=[('qd', i_)], dma=True)
            qd_sq = [('qd', i_) for i_ in range(8)]
            qd_iq = [('qd', 8 + i_) for i_ in range(8)]

            def bg_block(qb):
                q0 = qb * QB_DSA
                S.add('sp', lambda e: e.dma_start(out=iqT, in_=iq_d[:, :, q0:q0 + QB_DSA].rearrange("h p t -> p h t")),
                      r=qd_iq, w=[('iqT', pair) for pair in range(8)], dma=True)
                for ql in range(NQT):
                    qt = qb * NQT + ql
                    nk = (qt + 1) * 128
                    nch = (nk + 511) // 512
                    for h in range(16):
                        pair, half = h // 2, h % 2
                        for ch in range(nch):
                            n = min(512, nk - ch * 512)
                            pbk = (3, 5)[(h * nch + ch) % 2]
                            mm(bank(pbk)[:, :n], iqT[half * 64:(half + 1) * 64, pair, ql * 128:(ql + 1) * 128],
                               ikT2[half * 64:(half + 1) * 64, ch * 512:ch * 512 + n], True, True,
                               r=[('iqT', pair), 'ikT2'], w=[('ps', pbk)])
                            rt = rtmp[(h * nch + ch) % 2]
                            rk = ('rtmp', (h * nch + ch) % 2)
                            S.add('act', lambda e, pbk=pbk, n=n, rt=rt, qt=qt, h=h: e.activation(
                                out=rt[:, :n], in_=bank(pbk)[:, :n], func=AF.Relu, scale=iwa[:, qt, h:h + 1]),
                                r=[('ps', pbk), 'iwa'], w=[rk])
                            if h == 0:
                                S.add('dve', lambda e, n=n, rt=rt, ch=ch, qt=qt, h=h: e.tensor_scalar(
                                    out=isc[:, ch * 512:ch * 512 + n], in0=rt[:, :n], scalar1=iws[:, qt, h:h + 1],
                                    scalar2=None, op0=ALU.mult),
                                    r=[rk, 'iws'], w=[('isc', ch)])
                            else:
                                S.add('dve', lambda e, n=n, rt=rt, ch=ch, qt=qt, h=h: e.scalar_tensor_tensor(
                                    out=isc[:, ch * 512:ch * 512 + n], in0=rt[:, :n], scalar=iws[:, qt, h:h + 1],
                                    in1=isc[:, ch * 512:ch * 512 + n], op0=ALU.mult, op1=ALU.add),
                                    r=[rk, 'iws', ('isc', ch)], w=[('isc', ch)])
                    sb_ = selb[ql]
                    if qt >= 2:
                        S.add('dve', lambda e, nk=nk: e.tensor_reduce(out=bs[:, 0:1], in_=isc[:, :nk], axis=AX.X, op=ALU.max,
                                                                      apply_absolute_value=True),
                              r=isc_all, w=['bs_c'])
                        S.add('dve', lambda e: e.tensor_scalar(out=bs[:, 1:2], in0=bs[:, 0:1], scalar1=2.0, scalar2=2.0,
                                                               op0=ALU.mult, op1=ALU.add), r=['bs_c'], w=['bs_w'])
                        S.add('dve', lambda e: e.tensor_scalar(out=bs[:, 2:3], in0=bs[:, 0:1], scalar1=-1.0, scalar2=-1.0,
                                                               op0=ALU.mult, op1=ALU.add), r=['bs_c'], w=['bs_lo'])
                    S.add('pool', lambda e, qt=qt: e.affine_select(
                        out=isc[:, qt * 128:(qt + 1) * 128], in_=isc[:, qt * 128:(qt + 1) * 128],
                        pattern=[[-1, 128]], compare_op=ALU.is_ge, fill=NEG_MASK, base=0, channel_multiplier=1),
                        r=isc_all + ['bs_c'], w=isc_all)
                    if qt >= 2:
                        for it in range(NBIS):
                            ck = 0.5 ** (it + 1)
                            S.add('dve', lambda e, ck=ck: e.scalar_tensor_tensor(
                                out=bs[:, 3:4], in0=bs[:, 1:2], scalar=ck, in1=bs[:, 2:3], op0=ALU.mult, op1=ALU.add),
                                r=['bs_w', 'bs_lo'], w=['bs_mid'])
                            S.add('dve', lambda e, nk=nk, sb_=sb_: e.tensor_scalar(
                                out=sb_[:, :nk], in0=isc[:, :nk], scalar1=bs[:, 3:4], scalar2=0.0, op0=ALU.is_ge,
                                op1=ALU.add, accum_out=bs[:, 4:5]),
                                r=isc_all + ['bs_mid'], w=[('selb', ql), 'bs_cnt'])
                            S.add('dve', lambda e: e.tensor_scalar(
                                out=bs[:, 5:6], in0=bs[:, 4:5], scalar1=255.5, scalar2=bs[:, 1:2], op0=ALU.is_ge,
                                op1=ALU.mult), r=['bs_cnt', 'bs_w'], w=['bs_g'])
                            S.add('dve', lambda e, ck=ck: e.scalar_tensor_tensor(
                                out=bs[:, 2:3], in0=bs[:, 5:6], scalar=ck, in1=bs[:, 2:3], op0=ALU.mult, op1=ALU.add),
                                r=['bs_g', 'bs_lo'], w=['bs_lo'])
                        S.add('dve', lambda e, nk=nk, sb_=sb_: e.tensor_scalar(out=sb_[:, :nk], in0=isc[:, :nk],
                                                                               scalar1=bs[:, 2:3], scalar2=None, op0=ALU.is_ge),
                              r=isc_all + ['bs_lo'], w=[('selb', ql)])
                    else:
                        S.add('dve', lambda e, nk=nk, sb_=sb_: e.tensor_scalar(out=sb_[:, :nk], in0=isc[:, :nk], scalar1=-1.0e29,
                                                                               scalar2=None, op0=ALU.is_ge),
                              r=isc_all, w=[('selb', ql)])

            def build_selT(qb):
                for ql in range(NQT):
                    qt = qb * NQT + ql
                    sb_ = selb[ql]
                    for g in range((qt + 4) // 4):
                        pb = 6 + rr['cp'] % 2
                        pbv = bank(pb).bitcast(BF16)
                        cnt = min(4, qt + 1 - g * 4)
                        for kk in range(cnt):
                            stile = g * 4 + kk
                            S.add('pe', lambda e, pbv=pbv, kk=kk, stile=stile, sb_=sb_: e.transpose(
                                out=pbv[:, kk * 128:(kk + 1) * 128], in_=sb_[:, stile * 128:(stile + 1) * 128],
                                identity=ident_b), r=[('selb', ql), 'ident_b'], w=[('ps', pb)])
                        evac(selT[:, g * 4:g * 4 + cnt, ql * 128:(ql + 1) * 128],
                             pbv[:, :cnt * 128].rearrange("p (a c) -> p a c", a=cnt),
                             r=[('ps', pb)], w=['selT'])

            def record(fn):
                rec = []
                orig = S.add
                S.add = lambda *a_, **k_: rec.append((a_, k_))
                try:
                    fn()
                finally:
                    S.add = orig
                return rec

            bg_block(0)
            for qb in range(NQB):
                q0 = qb * QB_DSA
                build_selT(qb)
                rec = record(lambda: bg_block(qb + 1)) if qb + 1 < NQB else []
                n_items = 0
                first_tile = q0 // 128
                for h in range(8):
                    for stile in range(0, first_tile + NQT):
                        if stile < first_tile and SLOPES_DSA[h] * 128.0 * (first_tile - stile - 1) > SKIP_EXP:
                            continue
                        n_items += 1
                per = (len(rec) + n_items - 1) // n_items if rec else 0
                rstate = {'i': 0}

                def hook():
                    for _ in range(per):
                        if rstate['i'] < len(rec):
                            a_, k_ = rec[rstate['i']]
                            S.add(*a_, **k_)
                            rstate['i'] += 1
                S.add('sp', lambda e, q0=q0: e.dma_start(out=sq_blk, in_=sq_d[:, :, q0:q0 + QB_DSA].rearrange("h p t -> p h t")),
                      r=qd_sq, w=['sq_blk'], dma=True)
                for h in range(8):
                    par = h % 2
                    slope = SLOPES_DSA[h]
                    make_T(slope, par)
                    sq_cur = sq_blk[:, h, :]

                    def kq(stile, col0, n, sq_cur=sq_cur, h=h):
                        return (skT[:, stile * 128:(stile + 1) * 128], sq_cur[:, col0:col0 + n],
                                ['skT', 'sq_blk'])
                    attn_block(slope, par, q0, QB_DSA, [kq],
                               lambda stile: (sv[:, stile, :], 'sv'),
                               lambda stile, col0, n: (selT[:, stile, col0:col0 + n], 'selT'),
                               scale_s, [2], [4], 1, hook=hook)
                    recip_z(0, 4, QB_DSA)
                    S.add('dve', lambda e, h=h, q0=q0: e.tensor_tensor(
                        out=catT[:, 8 + h, q0:q0 + QB_DSA], in0=bank(2)[:, :QB_DSA], in1=lnz[0][:, :QB_DSA],
                        op=ALU.mult), r=[('ps', 2), ('lnz', 0)], w=[('catT', 8 + h)])
                while rstate['i'] < len(rec):
                    a_, k_ = rec[rstate['i']]
                    S.add(*a_, **k_)
                    rstate['i'] += 1

            if stage == "dbgA3":
                S.add('sp', lambda e: e.dma_start(out=dbg, in_=big2.rearrange("p a b -> p (a b)")), r=[('catT', hh) for hh in range(16)], dma=True)
                raise _Stop()
            ar.reset(work0)
            xs = [ar.alloc(F32, 2048) for _ in range(2)]
            lng = ar.alloc(F32, 2048)
            lnb = ar.alloc(F32, 2048)
            rb = [ar.alloc(F32, 2048) for _ in range(2)]
            st6 = ar.alloc(F32, 4, 6)
            mv = ar.alloc(F32, 2)
            rstd = ar.alloc(F32, 1)
            wo_b = big1
            cat_all = [('catT', hh) for hh in range(16)]
            for kq4 in range(4):
                S.add('pool', lambda e, kq4=kq4: e.dma_start(
                    out=wo_b[:, kq4 * 4:(kq4 + 1) * 4, :],
                    in_=w_o.rearrange("(k p) c -> p k c", p=128)[:, kq4 * 4:(kq4 + 1) * 4, :]),
                    r=cat_all, w=xT_all + ['wo_b'], dma=True)
            S.add('sp', lambda e: e.dma_start(out=lng, in_=ln1_g[0:1, :].to_broadcast([128, 2048])),
                  r=cat_all, w=['lng'], dma=True)
            S.add('sp', lambda e: e.dma_start(out=lnb, in_=ln1_b[0:1, :].to_broadcast([128, 2048])),
                  r=cat_all, w=['lnb'], dma=True)
            for tt in range(NTT):
                xb = xs[tt % 2]
                rbuf = rb[tt % 2]
                S.add('sp', lambda e, xb=xb, tt=tt: e.dma_start(out=xb, in_=x[b, tt * 128:(tt + 1) * 128, :]),
                      r=cat_all, w=[('xs', tt % 2)], dma=True)
                for cb in range(4):
                    pbo = 4 * (tt % 2) + cb
                    for k in range(16):
                        mm(bank(pbo), catT[:, k, tt * 128:(tt + 1) * 128], wo_b[:, k, cb * 512:(cb + 1) * 512],
                           k == 0, k == 15, r=cat_all + ['wo_b'] + xT_all, w=[('ps', pbo)])
                    S.add('dve', lambda e, cb=cb, xb=xb, rbuf=rbuf, pbo=pbo: e.scalar_tensor_tensor(
                        out=rbuf[:, cb * 512:(cb + 1) * 512], in0=xb[:, cb * 512:(cb + 1) * 512], scalar=ALPHA,
                        in1=bank(pbo), op0=ALU.mult, op1=ALU.add),
                        r=[('xs', tt % 2), ('ps', pbo)], w=[('rb', tt % 2, cb)])
                    S.add('dve', lambda e, cb=cb, rbuf=rbuf: e.bn_stats(out=st6[:, cb, :], in_=rbuf[:, cb * 512:(cb + 1) * 512]),
                          r=[('rb', tt % 2, cb)], w=[('st6', cb)])
                rb_all = [('rb', tt % 2, cb) for cb in range(4)]
                S.add('dve', lambda e: e.bn_aggr(out=mv, in_=st6), r=[('st6', cb) for cb in range(4)], w=['mv'])
                S.add('act', lambda e: e.activation(out=rstd, in_=mv[:, 1:2], func=AF.Sqrt, bias=eps_ln),
                      r=['mv', 'eps'], w=['rstd'])
                S.add('dve', lambda e: e.reciprocal(out=rstd, in_=rstd), r=['rstd'], w=['rstd'])
                S.add('dve', lambda e, rbuf=rbuf: e.tensor_scalar(out=rbuf, in0=rbuf, scalar1=mv[:, 0:1], scalar2=rstd,
                                                                   op0=ALU.subtract, op1=ALU.mult),
                      r=rb_all + ['mv', 'rstd'], w=rb_all)
                S.add('pool', lambda e, rbuf=rbuf: e.tensor_tensor(out=rbuf, in0=rbuf, in1=lng, op=ALU.mult),
                      r=rb_all + ['lng'], w=rb_all)
                S.add('pool', lambda e, rbuf=rbuf: e.tensor_tensor(out=rbuf, in0=rbuf, in1=lnb, op=ALU.add),
                      r=rb_all + ['lnb'], w=rb_all)
                row0 = b * S_LEN + tt * 128
                dst = out if stage == "A" else x1s
                S.add('sp', lambda e, rbuf=rbuf, row0=row0, dst=dst: e.dma_start(out=dst[row0:row0 + 128, :], in_=rbuf),
                      r=rb_all, w=[('x1s', b, tt)], dma=True)


        def phase_b():
            NG = NSEQ * NTT
            gateB = [('x1s', bb, tt) for bb in range(NSEQ) for tt in range(NTT)]
            while tbl_state['i'] < len(tbl_chunks):
                emit_tbl_chunk()
            tbl_all = [('tbl', i) for i in range(len(tbl_chunks))]
            xT_all = [('xT', tt) for tt in range(NTT)]
            cat_all = [('catT', hh) for hh in range(16)]
            ar.reset(workB0)
            xs1 = ar.alloc(F32, 2048)
            x1b = [ar.alloc(BF16, 2048) for _ in range(2)]
            off_xq = ar.off
            x1T = ar.alloc(BF16, 16, 128)
            qT = ar.alloc(BF16, 16, 128)
            cand = ar.t[:, off_xq:off_xq + 2048].rearrange("p (h c) -> p h c", h=8)
            s_sb = ar.alloc(F32, 16, 128)
            lng = ar.alloc(F32, 2048)
            lnb = ar.alloc(F32, 2048)
            rb = ar.alloc(F32, 2048)
            kT = [ar.alloc(BF16, 128) for _ in range(2)]
            m16 = ar.alloc(F32, 16, 16)
            i16 = ar.alloc(U32, 16, 16)
            i16f = ar.alloc(F32, 16, 16)
            ts = ar.alloc(F32, 8, 16)
            pos = ar.alloc(U32, 8, 16)
            pa = ar.alloc(U32, 128)
            pb_ = ar.alloc(U32, 128)
            paf = ar.alloc(F32, 128)
            pbf = ar.alloc(F32, 128)
            red1 = ar.alloc(F32, 128)
            red2 = ar.alloc(F32, 128)
            iota16 = ar.alloc(F32, 16)
            eidx = [ar.alloc(I32, 128) for _ in range(2)]
            gts = [ar.alloc(F32, 8, 16) for _ in range(2)]
            negm = ar.alloc(F32, 8)
            zs = ar.alloc(F32, 8)
            hv = ar.alloc(F32, 128)
            av = ar.alloc(F32, 128)
            dg = [ar.alloc(BF16, 128) for _ in range(4)]
            st6 = ar.alloc(F32, 4, 6)
            mv = ar.alloc(F32, 2)
            rstd = ar.alloc(F32, 1)
            wq_b = big1
            ring = big2.rearrange("p (s two) d -> p s (two d)", two=2)

            for kq4 in range(4):
                S.add('pool', lambda e, kq4=kq4: e.dma_start(
                    out=wq_b[:, kq4 * 4:(kq4 + 1) * 4, :],
                    in_=peer_wq.rearrange("(k p) c -> p k c", p=128)[:, kq4 * 4:(kq4 + 1) * 4, :]),
                    r=gateB, w=xT_all + ['wo_b'], dma=True)
            S.add('sp', lambda e: e.dma_start(out=lng, in_=ln2_g[0:1, :].to_broadcast([128, 2048])),
                  r=gateB, w=['lng'], dma=True)
            S.add('sp', lambda e: e.dma_start(out=lnb, in_=ln2_b[0:1, :].to_broadcast([128, 2048])),
                  r=gateB, w=['lnb'], dma=True)
            S.add('pool', lambda e: e.iota(iota16, [[1, 16]], base=0, channel_multiplier=0,
                                           allow_small_or_imprecise_dtypes=True), r=gateB, w=['iota16'])
            for i, kk in enumerate((peer_k1, peer_k2)):
                S.add('sp', lambda e, kk=kk: e.dma_start(out=xs1[:, 0:128], in_=kk[:, :]),
                      r=gateB, w=['xs1', 'val', 'valc'] + [('lamv', q_) for q_ in range(4)] + [('lamt', q_) for q_ in range(2)],
                      dma=True)
                S.add('pe', lambda e: e.transpose(out=bank(6)[:, 0:128], in_=xs1[:, 0:128], identity=ident_f),
                      r=['xs1', 'ident_f'], w=[('ps', 6)])
                S.add('dve', lambda e, i=i: e.tensor_copy(out=kT[i], in_=bank(6)[:, 0:128]),
                      r=[('ps', 6)], w=[('kT', i)])

            def stage1(g):
                b_, tt = g // NTT, g % NTT
                p2 = g % 2
                S.add('sp', lambda e: e.dma_start(out=xs1, in_=x1s[g * 128:(g + 1) * 128, :]),
                      r=[('x1s', b_, tt)], w=['xs1'], dma=True)
                S.add('act', lambda e: e.activation(out=x1b[p2], in_=xs1, func=AF.Copy),
                      r=['xs1'], w=[('x1b', p2)])
                for gg in range(4):
                    pbk = 6 + gg % 2
                    for kk in range(4):
                        k = gg * 4 + kk
                        S.add('pe', lambda e, pbk=pbk, kk=kk, k=k: e.transpose(
                            out=bank(pbk)[:, kk * 128:(kk + 1) * 128], in_=xs1[:, k * 128:(k + 1) * 128],
                            identity=ident_f), r=['xs1', 'ident_f'], w=[('ps', pbk)])
                    evac(x1T[:, gg * 4:(gg + 1) * 4, :], bank(pbk).rearrange("p (a c) -> p a c", a=4),
                         r=[('ps', pbk)], w=['x1T'])
                for gg in range(4):
                    pbk = 6 + gg % 2
                    for jj in range(4):
                        j = gg * 4 + jj
                        for k in range(16):
                            mm(bank(pbk)[:, jj * 128:(jj + 1) * 128], wq_b[:, k, j * 128:(j + 1) * 128],
                               x1T[:, k, :], k == 0, k == 15, r=['x1T', 'wo_b'] + xT_all, w=[('ps', pbk)])
                    evac(qT[:, gg * 4:(gg + 1) * 4, :], bank(pbk).rearrange("p (a c) -> p a c", a=4),
                         r=[('ps', pbk)], w=['qT'])
                for gg in range(4):
                    pbk = gg % 2
                    for jj in range(4):
                        j = gg * 4 + jj
                        mm(bank(pbk)[:, jj * 128:(jj + 1) * 128], qT[:, j, :], kT[j % 2], True, True,
                           r=['qT', ('kT', j % 2)], w=[('ps', pbk)])
                    evac(s_sb[:, gg * 4:(gg + 1) * 4, :], bank(pbk).rearrange("p (a c) -> p a c", a=4),
                         r=[('ps', pbk)], w=['s_sb'])
                for j in range(16):
                    S.add('dve', lambda e, j=j: e.max(out=m16[:, j, 0:8], in_=s_sb[:, j, :]), r=['s_sb'], w=['m16'])
                    S.add('dve', lambda e, j=j: e.max_index(out=i16[:, j, 0:8], in_max=m16[:, j, 0:8],
                                                            in_values=s_sb[:, j, :]), r=['s_sb', 'm16'], w=['i16'])
                    S.add('dve', lambda e, j=j: e.match_replace(out=s_sb[:, j, :], in_to_replace=m16[:, j, 0:8],
                                                                in_values=s_sb[:, j, :], imm_value=NEG_SEL),
                          r=['s_sb', 'm16'], w=['s_sb'])
                    S.add('dve', lambda e, j=j: e.max(out=m16[:, j, 8:16], in_=s_sb[:, j, :]), r=['s_sb'], w=['m16'])
                    S.add('dve', lambda e, j=j: e.max_index(out=i16[:, j, 8:16], in_max=m16[:, j, 8:16],
                                                            in_values=s_sb[:, j, :]), r=['s_sb', 'm16'], w=['i16'])
                m16v = m16.rearrange("p (h c) k -> p h c k", c=2)
                S.add('dve', lambda e: e.tensor_tensor(
                    out=cand.rearrange("p h (a b) -> p h a b", a=16),
                    in0=m16v[:, :, 0, :].unsqueeze(3).to_broadcast([128, 8, 16, 16]),
                    in1=m16v[:, :, 1, :].unsqueeze(2).to_broadcast([128, 8, 16, 16]), op=ALU.add),
                    r=['m16'], w=['x1T', 'qT'])
                S.add('dve', lambda e: e.tensor_copy(out=i16f, in_=i16), r=['i16'], w=['i16f'])
                i16v = i16f.rearrange("p (h c) k -> p h c k", c=2)
                S.add('dve', lambda e: e.tensor_scalar(out=i16v[:, :, 0, :], in0=i16v[:, :, 0, :], scalar1=128.0,
                                                       scalar2=None, op0=ALU.mult), r=['i16f'], w=['i16f'])
                for h in range(8):
                    S.add('dve', lambda e, h=h: e.max(out=ts[:, h, 0:8], in_=cand[:, h, :]), r=['x1T', 'qT'], w=['ts'])
                    S.add('dve', lambda e, h=h: e.max_index(out=pos[:, h, 0:8], in_max=ts[:, h, 0:8],
                                                            in_values=cand[:, h, :]), r=['x1T', 'qT', 'ts'], w=['pos'])
                    S.add('dve', lambda e, h=h: e.match_replace(out=cand[:, h, :], in_to_replace=ts[:, h, 0:8],
                                                                in_values=cand[:, h, :], imm_value=NEG_SEL),
                          r=['x1T', 'qT', 'ts'], w=['x1T', 'qT'])
                    S.add('dve', lambda e, h=h: e.max(out=ts[:, h, 8:16], in_=cand[:, h, :]), r=['x1T', 'qT'], w=['ts'])
                    S.add('dve', lambda e, h=h: e.max_index(out=pos[:, h, 8:16], in_max=ts[:, h, 8:16],
                                                            in_values=cand[:, h, :]), r=['x1T', 'qT', 'ts'], w=['pos'])
                posf = pos.rearrange("p h k -> p (h k)")
                S.add('dve', lambda e: e.tensor_single_scalar(out=pa, in_=posf, scalar=4, op=ALU.logical_shift_right),
                      r=['pos'], w=['pa'])
                S.add('dve', lambda e: e.tensor_single_scalar(out=pb_, in_=posf, scalar=15, op=ALU.bitwise_and),
                      r=['pos'], w=['pb'])
                S.add('dve', lambda e: e.tensor_copy(out=paf, in_=pa), r=['pa'], w=['paf'])
                S.add('dve', lambda e: e.tensor_copy(out=pbf, in_=pb_), r=['pb'], w=['pbf'])
                oh = s_sb.rearrange("p a b -> p (a b)").rearrange("p (m k) -> p m k", k=16)
                oh4 = s_sb.rearrange("p a b -> p (a b)").rearrange("p (h k a) -> p h k a", h=8, k=16)
                for (src, half, red) in ((paf, 0, red1), (pbf, 1, red2)):
                    S.add('dve', lambda e, src=src: e.tensor_tensor(
                        out=oh, in0=src.unsqueeze(2).to_broadcast([128, 128, 16]),
                        in1=iota16.unsqueeze(1).to_broadcast([128, 128, 16]), op=ALU.is_equal),
                        r=['paf', 'pbf', 'iota16', 's_sb'], w=['s_sb'])
                    S.add('dve', lambda e, half=half: e.tensor_tensor(
                        out=oh4, in0=oh4, in1=i16v[:, :, half, :].unsqueeze(2).to_broadcast([128, 8, 16, 16]),
                        op=ALU.mult), r=['s_sb', 'i16f'], w=['s_sb'])
                    S.add('dve', lambda e, red=red: e.tensor_reduce(out=red, in_=oh, axis=AX.X, op=ALU.add),
                          r=['s_sb'], w=[('red', half)])
                S.add('dve', lambda e: e.tensor_tensor(out=red1, in0=red1, in1=red2, op=ALU.add),
                      r=[('red', 0), ('red', 1)], w=[('red', 0)])
                S.add('dve', lambda e: e.tensor_copy(out=eidx[p2], in_=red1), r=[('red', 0)], w=[('eidx', p2)])
                S.add('dve', lambda e: e.tensor_scalar(out=negm, in0=ts[:, :, 0], scalar1=-1.0, scalar2=None, op0=ALU.mult),
                      r=['ts'], w=['negm'])
                for h in range(8):
                    S.add('act', lambda e, h=h: e.activation(out=gts[p2][:, h, :], in_=ts[:, h, :], func=AF.Exp,
                                                             bias=negm[:, h:h + 1], accum_out=zs[:, h:h + 1]),
                          r=['ts', 'negm'], w=[('gts', p2), 'zs'])
                S.add('dve', lambda e: e.reciprocal(out=zs, in_=zs), r=['zs'], w=['zs'])
                S.add('dve', lambda e: e.tensor_tensor(out=gts[p2], in0=gts[p2],
                                                       in1=zs.unsqueeze(2).to_broadcast([128, 8, 16]), op=ALU.mult),
                      r=[('gts', p2), 'zs'], w=[('gts', p2)])

            ring_n = {'i': 0}
            LA = 5

            def tile_items(g):
                p2 = g % 2
                GS = 2

                def mk_u(slot):
                    def gat():
                        rs = ring_n['i'] % 8
                        ring_n['i'] += 1
                        rkeys = [('catT', 2 * rs), ('catT', 2 * rs + 1)]
                        S.add('pool', lambda e: e.indirect_dma_start(
                            out=ring[:, rs, :], out_offset=None, in_=uvbf[:, :],
                            in_offset=bass.IndirectOffsetOnAxis(ap=eidx[p2][:, slot:slot + 1], axis=0)),
                            r=[('eidx', p2)] + tbl_all, w=rkeys, dma=True)
                        return rs

                    def con(rs):
                        ring_of[slot] = rs
                        rkeys = [('catT', 2 * rs), ('catT', 2 * rs + 1)]
                        S.add('dve', lambda e: e.tensor_tensor(
                            out=ring[:, rs, 0:D], in0=ring[:, rs, 0:D], in1=x1b[p2], op=ALU.mult),
                            r=rkeys + [('x1b', p2)], w=[rkeys[0]])
                        S.add('act', lambda e: e.activation(
                            out=ring[:, rs, 0:D], in_=ring[:, rs, 0:D], func=AF.Copy,
                            accum_out=hv[:, slot:slot + 1]),
                            r=[rkeys[0]], w=[rkeys[0], ('hv', slot // GS)])
                    return (gat, con)

                def mk_av(grp):
                    def con(_):
                        sl_ = slice(grp * GS, (grp + 1) * GS)
                        S.add('act', lambda e: e.activation(out=av[:, sl_], in_=hv[:, sl_], func=AF.Gelu),
                              r=[('hv', grp)], w=[('av', grp)])
                        S.add('dve', lambda e: e.tensor_tensor(
                            out=av[:, sl_], in0=av[:, sl_], in1=gts[p2].rearrange("p h k -> p (h k)")[:, sl_],
                            op=ALU.mult), r=[('av', grp), ('gts', p2)], w=[('av', grp)])
                        for slot in range(grp * GS, (grp + 1) * GS):
                            rs = ring_of[slot]
                            rkeys = [('catT', 2 * rs), ('catT', 2 * rs + 1)]
                            d = dg[slot % 4]
                            S.add('act', lambda e, d=d, slot=slot: e.activation(
                                out=d, in_=ident_b, func=AF.Copy, scale=av[:, slot:slot + 1]),
                                r=['ident_b', ('av', grp)], w=[('dg', slot % 4)])
                            for cb in range(4):
                                mm(bank(2 + cb), d, ring[:, rs, D + cb * 512:D + (cb + 1) * 512], slot == 0, slot == 127,
                                   r=[('dg', slot % 4)] + rkeys, w=[('ps', 2 + cb)])
                    return (None, con)

                ring_of = {}
                ngrp = 128 // GS
                items = [mk_u(sl_) for sl_ in range(GS)]
                for grp in range(ngrp):
                    if grp + 1 < ngrp:
                        items.extend(mk_u(sl_) for sl_ in range((grp + 1) * GS, (grp + 2) * GS))
                    items.append(mk_av(grp))
                items.append((None, lambda _: fin(g)))
                return items

            def fin(g):
                S.add('sp', lambda e: e.dma_start(out=rb, in_=x1s[g * 128:(g + 1) * 128, :]), w=['rb'], dma=True)
                for cb in range(4):
                    S.add('dve', lambda e, cb=cb: e.scalar_tensor_tensor(
                        out=rb[:, cb * 512:(cb + 1) * 512], in0=rb[:, cb * 512:(cb + 1) * 512], scalar=ALPHA,
                        in1=bank(2 + cb), op0=ALU.mult, op1=ALU.add), r=['rb', ('ps', 2 + cb)], w=['rb'])
                    S.add('dve', lambda e, cb=cb: e.bn_stats(out=st6[:, cb, :], in_=rb[:, cb * 512:(cb + 1) * 512]),
                          r=['rb'], w=['st6'])
                S.add('dve', lambda e: e.bn_aggr(out=mv, in_=st6), r=['st6'], w=['mv'])
                S.add('act', lambda e: e.activation(out=rstd, in_=mv[:, 1:2], func=AF.Sqrt, bias=eps_ln),
                      r=['mv', 'eps'], w=['rstd'])
                S.add('dve', lambda e: e.reciprocal(out=rstd, in_=rstd), r=['rstd'], w=['rstd'])
                S.add('dve', lambda e: e.tensor_scalar(out=rb, in0=rb, scalar1=mv[:, 0:1], scalar2=rstd,
                                                       op0=ALU.subtract, op1=ALU.mult), r=['rb', 'mv', 'rstd'], w=['rb'])
                S.add('dve', lambda e: e.tensor_tensor(out=rb, in0=rb, in1=lng, op=ALU.mult), r=['rb', 'lng'], w=['rb'])
                S.add('dve', lambda e: e.tensor_tensor(out=rb, in0=rb, in1=lnb, op=ALU.add), r=['rb', 'lnb'], w=['rb'])
                S.add('sp', lambda e: e.dma_start(out=out[g * 128:(g + 1) * 128, :], in_=rb), r=['rb'], w=[('out', g)], dma=True)

            def record_stage1(g):
                rec = []
                orig = S.add
                S.add = lambda *a_, **k_: rec.append((a_, k_))
                try:
                    stage1(g)
                finally:
                    S.add = orig
                return rec

            ngl = NG if stage == "full" else int(stage[2:])
            stage1(0)
            for g in range(ngl):
                items = tile_items(g)
                rec = record_stage1(g + 1) if g + 1 < ngl else []
                gl = [i for i, it in enumerate(items) if it[0] is not None]
                per = (len(rec) + 199) // 200 if rec else 0
                slots = {}
                gi = 0
                ri = 0
                ngat = 0
                for ci, (gf, cf) in enumerate(items):
                    while gi < len(gl) and ngat < 8:
                        slots[gl[gi]] = items[gl[gi]][0]()
                        gi += 1
                        ngat += 1
                    if gf is not None:
                        cf(slots.pop(ci))
                    else:
                        cf(None)
                        if ci < len(items) - 1:
                            ngat -= 2
                    if ci >= 8:
                        for _ in range(per):
                            if ri < len(rec):
                                a_, k_ = rec[ri]
                                S.add(*a_, **k_)
                                ri += 1
                while ri < len(rec):
                    a_, k_ = rec[ri]
                    S.add(*a_, **k_)
                    ri += 1

        try:
            for b in range(NSEQ):
                if not stage.startswith("Bo"):
                    phase_a(b)
            if stage == "full" or stage.startswith("B"):
                phase_b()
        except _Stop:
            pass

        S.finalize(block, st)
        print("ops per engine:", S.stats, flush=True)
    return nc


_CACHE = {}


def kernel(**inputs):
    stage = inputs.pop("_stage", "full")
    n = 8
    x = np.ascontiguousarray(inputs["x"], dtype=np.float32)
    shared = {}
    for k in ("w_in", "w_o", "lambda_q1", "lambda_k1", "lambda_q2", "lambda_k2", "subln_g", "ln1_g", "ln1_b",
              "peer_wq", "peer_k1", "peer_k2", "peer_u", "peer_v", "ln2_g", "ln2_b"):
        shared[k] = np.ascontiguousarray(np.asarray(inputs[k], dtype=np.float32)[0])
    for k in ("lambda_q1", "lambda_k1", "lambda_q2", "lambda_k2", "subln_g", "ln1_g", "ln1_b", "ln2_g", "ln2_b"):
        shared[k] = shared[k].reshape(1, -1)
    if stage not in _CACHE:
        _CACHE[stage] = build_program(stage)
    nc = _CACHE[stage]
    in_maps = []
    for c in range(n):
        m = dict(shared)
        m["x"] = x[c * NSEQ:(c + 1) * NSEQ]
        in_maps.append(m)
    res = run_bass_kernel_spmd(nc, in_maps, core_ids=list(range(n)))
    outs = [np.asarray(r["out"]).reshape(NSEQ, S_LEN, D) for r in res.results]
    return np.concatenate(outs, axis=0).astype(np.float32)
```
